# Optimizing a Trainium2 kernel written in Bass

```python
import jax
import jax.numpy as jnp
from jax import lax
import numpy as np

D_MODEL = 1024
BATCH = 8
SEQ = 2048
DEPTH = 1

MEM_LEN = 256
GRID_W = 64
EPS = 1e-6
D_MIX = D_MODEL
ATT_HEADS = 8
ATT_KV_HEADS = 2
HEAD_DIM = 64
D_ATT = ATT_HEADS * HEAD_DIM
D_KV = ATT_KV_HEADS * HEAD_DIM
Q_BLOCK = 128
ROPE_THETA = 10000.0
D_SSM = D_MIX - D_ATT
SSM_GROUP = 16
SSM_GROUPS = D_SSM // SSM_GROUP
SSM_STATE = 64
DT_MIN = 1e-3
DT_MAX = 1e-1
D_IN = D_ATT + 2 * D_KV + D_SSM
XA_HEADS = 4
XA_HEAD_DIM = D_MODEL // XA_HEADS
N_EXPERT_GROUPS = 4
EXPERTS_PER_GROUP = 4
N_EXPERTS = N_EXPERT_GROUPS * EXPERTS_PER_GROUP
TOP_K = 2
D_EXPERT = D_MODEL // 2

kernel_name = "hybrid_attn_s5_xattn_hmoe_encoder"


def rmsnorm(x, g):
    xf = x.astype(jnp.float32)
    y = xf * lax.rsqrt(jnp.mean(xf * xf, axis=-1, keepdims=True) + EPS)
    return y.astype(x.dtype) * g


def rope_1d(x, pos):
    half = x.shape[-1] // 2
    freqs = ROPE_THETA ** (-jnp.arange(half, dtype=jnp.float32) / half)
    ang = pos.astype(jnp.float32)[:, None] * freqs[None, :]
    cos = jnp.cos(ang)[:, None, :].astype(x.dtype)
    sin = jnp.sin(ang)[:, None, :].astype(x.dtype)
    x1, x2 = x[..., :half], x[..., half:]
    return jnp.concatenate([x1 * cos - x2 * sin, x2 * cos + x1 * sin], axis=-1)


def axial_rope(x, row, col):
    half = HEAD_DIM // 2
    return jnp.concatenate([rope_1d(x[..., :half], row), rope_1d(x[..., half:], col)], axis=-1)


def gqa_blocked(q, k, v):
    B, S, H, Dh = q.shape
    KV = k.shape[2]
    G = H // KV
    nb = S // Q_BLOCK
    qb = q.reshape(B, nb, Q_BLOCK, KV, G, Dh).transpose(1, 0, 2, 3, 4, 5)
    scale = Dh ** -0.5

    def one_block(q_blk):
        s = jnp.einsum('bqkgd,bskd->bkgqs', q_blk, k).astype(jnp.float32) * scale
        p = jax.nn.softmax(s, axis=-1).astype(v.dtype)
        return jnp.einsum('bkgqs,bskd->bqkgd', p, v)

    o = lax.map(one_block, qb)
    return o.transpose(1, 0, 2, 3, 4, 5).reshape(B, S, H * Dh)


def _complex_affine_combine(e1, e2):
    a1r, a1i, b1r, b1i = e1
    a2r, a2i, b2r, b2i = e2
    return (a2r * a1r - a2i * a1i,
            a2r * a1i + a2i * a1r,
            a2r * b1r - a2i * b1i + b2r,
            a2r * b1i + a2i * b1r + b2i)


def s5_direction(u, lam_re, lam_im, log_step, b_re, b_im, c_re, c_im, reverse):
    f32 = jnp.float32
    lr = jnp.minimum(lam_re.astype(f32), -1e-4)
    li = lam_im.astype(f32)
    dt = jnp.exp(log_step.astype(f32))[:, None]
    mag = jnp.exp(lr * dt)
    ab_r = mag * jnp.cos(li * dt)
    ab_i = mag * jnp.sin(li * dt)
    den = lr * lr + li * li
    nr = ab_r - 1.0
    f_r = (nr * lr + ab_i * li) / den
    f_i = (ab_i * lr - nr * li) / den
    br = b_re.astype(f32)
    bi = b_im.astype(f32)
    bb_r = f_r[..., None] * br - f_i[..., None] * bi
    bb_i = f_r[..., None] * bi + f_i[..., None] * br
    bu_r = jnp.einsum('bsgc,gpc->bsgp', u, bb_r)
    bu_i = jnp.einsum('bsgc,gpc->bsgp', u, bb_i)
    a_r = jnp.broadcast_to(ab_r, bu_r.shape)
    a_i = jnp.broadcast_to(ab_i, bu_i.shape)
    _, _, s_r, s_i = lax.associative_scan(
        _complex_affine_combine, (a_r, a_i, bu_r, bu_i), axis=1, reverse=reverse)
    return (jnp.einsum('bsgp,gcp->bsgc', s_r, c_re.astype(f32))
            - jnp.einsum('bsgp,gcp->bsgc', s_i, c_im.astype(f32)))


def s5_mixer(u, lam_re, lam_im, log_step, b_re, b_im, c_re, c_im, ssm_d, glu_w, glu_b):
    B, S, _ = u.shape
    uf = u.astype(jnp.float32)
    ug = uf.reshape(B, S, SSM_GROUPS, SSM_GROUP)
    y = (s5_direction(ug, lam_re[0], lam_im[0], log_step[0], b_re[0], b_im[0], c_re[0], c_im[0], False)
         + s5_direction(ug, lam_re[1], lam_im[1], log_step[1], b_re[1], b_im[1], c_re[1], c_im[1], True))
    y = (y.reshape(B, S, D_SSM) + ssm_d.astype(jnp.float32) * uf).astype(u.dtype)
    z = jax.nn.gelu(y)
    return z * jax.nn.sigmoid(z @ glu_w + glu_b)


def hybrid_mixer(x, norm_g, w_in, q_norm_g, k_norm_g, lam_re, lam_im, log_step, b_re, b_im,
                 c_re, c_im, ssm_d, glu_w, glu_b, att_out_g, ssm_out_g, w_out, row, col):
    B, S, _ = x.shape
    h = rmsnorm(x, norm_g)
    proj = h @ w_in
    q, k, v, u = jnp.split(proj, [D_ATT, D_ATT + D_KV, D_ATT + 2 * D_KV], axis=-1)
    q = rmsnorm(q.reshape(B, S, ATT_HEADS, HEAD_DIM), q_norm_g)
    k = rmsnorm(k.reshape(B, S, ATT_KV_HEADS, HEAD_DIM), k_norm_g)
    v = v.reshape(B, S, ATT_KV_HEADS, HEAD_DIM)
    q = axial_rope(q, row, col)
    k = axial_rope(k, row, col)
    y_att = gqa_blocked(q, k, v)
    y_ssm = s5_mixer(u, lam_re, lam_im, log_step, b_re, b_im, c_re, c_im, ssm_d, glu_w, glu_b)
    y = jnp.concatenate([rmsnorm(y_att, att_out_g), rmsnorm(y_ssm, ssm_out_g)], axis=-1)
    return y @ w_out


def memory_cross_attn(x, mem, xa_norm_g, mem_norm_g, wq, wk, wv, wo):
    B, S, _ = x.shape
    M = mem.shape[1]
    hq = rmsnorm(x, xa_norm_g)
    hm = rmsnorm(mem, mem_norm_g)
    q = (hq @ wq).reshape(B, S, XA_HEADS, XA_HEAD_DIM)
    k = (hm @ wk).reshape(B, M, XA_HEADS, XA_HEAD_DIM)
    v = (hm @ wv).reshape(B, M, XA_HEADS, XA_HEAD_DIM)
    s = jnp.einsum('bqhd,bmhd->bhqm', q, k).astype(jnp.float32) * (XA_HEAD_DIM ** -0.5)
    p = jax.nn.softmax(s, axis=-1).astype(v.dtype)
    o = jnp.einsum('bhqm,bmhd->bqhd', p, v).reshape(B, S, D_MODEL)
    return o @ wo


def hier_moe(x, norm_g, grp_w, grp_b, exp_w, exp_b, w_gate, w_up, w_down):
    B, S, D = x.shape
    T = B * S
    h = rmsnorm(x, norm_g).reshape(T, D)
    grp_prob = jax.nn.softmax((h @ grp_w).astype(jnp.float32) + grp_b, axis=-1)
    g_w, g_sel = lax.top_k(grp_prob, 1)
    exp_logits = ((h @ exp_w).astype(jnp.float32) + exp_b).reshape(T, N_EXPERT_GROUPS, EXPERTS_PER_GROUP)
    idx = jnp.broadcast_to(g_sel[:, :, None], (T, 1, EXPERTS_PER_GROUP))
    in_grp = jnp.take_along_axis(exp_logits, idx, axis=1)[:, 0]
    top_v, top_i = lax.top_k(in_grp, TOP_K)
    w = jax.nn.softmax(top_v, axis=-1) * g_w
    e_idx = g_sel * EXPERTS_PER_GROUP + top_i
    gates = jnp.sum(jax.nn.one_hot(e_idx, N_EXPERTS, dtype=jnp.float32) * w[..., None], axis=1)
    gates = gates.astype(h.dtype)
    y = jnp.zeros_like(h)
    for e in range(N_EXPERTS):
        a = jax.nn.silu(h @ w_gate[e]) * (h @ w_up[e])
        y = y + (a * gates[:, e:e + 1]) @ w_down[e]
    return y.reshape(B, S, D)


def setup_inputs(seed: int = 0) -> dict:
    key = jax.random.key(seed)
    ks = jax.random.split(key, 34)
    f32 = jnp.float32
    L = DEPTH

    def nrm(k, shape, scale):
        return jax.random.normal(k, shape, f32) * scale

    def gain(k, shape):
        return 1.0 + nrm(k, shape, 0.02)

    n_idx = jnp.arange(SSM_STATE, dtype=f32)
    lam_im0 = jnp.broadcast_to(jnp.pi * n_idx, (L, 2, SSM_GROUPS, SSM_STATE))
    return {
        'x': nrm(ks[0], (BATCH, SEQ, D_MODEL), 1.0),
        'mem': nrm(ks[1], (BATCH, MEM_LEN, D_MODEL), 1.0),
        'mix_norm_g': gain(ks[2], (L, D_MODEL)),
        'w_in': nrm(ks[3], (L, D_MODEL, D_IN), D_MODEL ** -0.5),
        'q_norm_g': gain(ks[4], (L, HEAD_DIM)),
        'k_norm_g': gain(ks[5], (L, HEAD_DIM)),
        'ssm_lambda_re': -0.5 + nrm(ks[6], (L, 2, SSM_GROUPS, SSM_STATE), 0.01),
        'ssm_lambda_im': lam_im0 + nrm(ks[7], (L, 2, SSM_GROUPS, SSM_STATE), 0.01),
        'ssm_log_step': jax.random.uniform(ks[8], (L, 2, SSM_GROUPS), f32,
                                           minval=float(np.log(DT_MIN)), maxval=float(np.log(DT_MAX))),
        'ssm_b_re': nrm(ks[9], (L, 2, SSM_GROUPS, SSM_STATE, SSM_GROUP), (2 * SSM_GROUP) ** -0.5),
        'ssm_b_im': nrm(ks[10], (L, 2, SSM_GROUPS, SSM_STATE, SSM_GROUP), (2 * SSM_GROUP) ** -0.5),
        'ssm_c_re': nrm(ks[11], (L, 2, SSM_GROUPS, SSM_GROUP, SSM_STATE), (2 * SSM_STATE) ** -0.5),
        'ssm_c_im': nrm(ks[12], (L, 2, SSM_GROUPS, SSM_GROUP, SSM_STATE), (2 * SSM_STATE) ** -0.5),
        'ssm_d': nrm(ks[13], (L, D_SSM), 1.0),
        'ssm_glu_w': nrm(ks[14], (L, D_SSM, D_SSM), D_SSM ** -0.5),
        'ssm_glu_b': nrm(ks[15], (L, D_SSM), 0.01),
        'att_out_g': gain(ks[16], (L, D_ATT)),
        'ssm_out_g': gain(ks[17], (L, D_SSM)),
        'w_out': nrm(ks[18], (L, D_MIX, D_MODEL), D_MIX ** -0.5),
        'xa_norm_g': gain(ks[19], (L, D_MODEL)),
        'mem_norm_g': gain(ks[20], (L, D_MODEL)),
        'xa_wq': nrm(ks[21], (L, D_MODEL, D_MODEL), D_MODEL ** -0.5),
        'xa_wk': nrm(ks[22], (L, D_MODEL, D_MODEL), D_MODEL ** -0.5),
        'xa_wv': nrm(ks[23], (L, D_MODEL, D_MODEL), D_MODEL ** -0.5),
        'xa_wo': nrm(ks[24], (L, D_MODEL, D_MODEL), D_MODEL ** -0.5),
        'moe_norm_g': gain(ks[25], (L, D_MODEL)),
        'router_grp_w': nrm(ks[26], (L, D_MODEL, N_EXPERT_GROUPS), D_MODEL ** -0.5),
        'router_grp_b': nrm(ks[27], (L, N_EXPERT_GROUPS), 0.01),
        'router_exp_w': nrm(ks[28], (L, D_MODEL, N_EXPERTS), D_MODEL ** -0.5),
        'router_exp_b': nrm(ks[29], (L, N_EXPERTS), 0.01),
        'exp_w_gate': nrm(ks[30], (L, N_EXPERTS, D_MODEL, D_EXPERT), D_MODEL ** -0.5),
        'exp_w_up': nrm(ks[31], (L, N_EXPERTS, D_MODEL, D_EXPERT), D_MODEL ** -0.5),
        'exp_w_down': nrm(ks[32], (L, N_EXPERTS, D_EXPERT, D_MODEL), D_EXPERT ** -0.5),
        'final_norm_g': gain(ks[33], (D_MODEL,)),
    }


def reference(x, mem, mix_norm_g, w_in, q_norm_g, k_norm_g, ssm_lambda_re, ssm_lambda_im,
              ssm_log_step, ssm_b_re, ssm_b_im, ssm_c_re, ssm_c_im, ssm_d, ssm_glu_w, ssm_glu_b,
              att_out_g, ssm_out_g, w_out, xa_norm_g, mem_norm_g, xa_wq, xa_wk, xa_wv, xa_wo,
              moe_norm_g, router_grp_w, router_grp_b, router_exp_w, router_exp_b,
              exp_w_gate, exp_w_up, exp_w_down, final_norm_g):
    S = x.shape[1]
    rows = S // GRID_W
    row = jnp.repeat(jnp.arange(rows, dtype=jnp.int32), GRID_W)
    col = jnp.tile(jnp.arange(GRID_W, dtype=jnp.int32), rows)
    for l in range(DEPTH):
        x = x + hybrid_mixer(x, mix_norm_g[l], w_in[l], q_norm_g[l], k_norm_g[l],
                             ssm_lambda_re[l], ssm_lambda_im[l], ssm_log_step[l],
                             ssm_b_re[l], ssm_b_im[l], ssm_c_re[l], ssm_c_im[l],
                             ssm_d[l], ssm_glu_w[l], ssm_glu_b[l],
                             att_out_g[l], ssm_out_g[l], w_out[l], row, col)
        x = x + memory_cross_attn(x, mem, xa_norm_g[l], mem_norm_g[l],
                                  xa_wq[l], xa_wk[l], xa_wv[l], xa_wo[l])
        x = x + hier_moe(x, moe_norm_g[l], router_grp_w[l], router_grp_b[l],
                         router_exp_w[l], router_exp_b[l],
                         exp_w_gate[l], exp_w_up[l], exp_w_down[l])
    return rmsnorm(x, final_norm_g)
```

```python
import numpy as np
from contextlib import ExitStack
import concourse.bass as bass
import concourse.mybir as mybir
from concourse.alu_op_type import AluOpType as ALU
from concourse.bass_utils import run_bass_kernel_spmd

AF = mybir.ActivationFunctionType
F32 = mybir.dt.float32
BF16 = mybir.dt.bfloat16
U8 = mybir.dt.uint8
AX = mybir.AxisListType
EPS = 1e-6


class Rec:
    __slots__ = ("eng", "fn", "deps", "signal", "semval", "is_dma", "dsem", "dval", "prev_dma")

    def __init__(self, eng, fn, deps, is_dma):
        self.eng = eng
        self.fn = fn
        self.deps = deps
        self.signal = False
        self.semval = 0
        self.is_dma = is_dma
        self.dsem = None
        self.dval = 0
        self.prev_dma = None


def _buf(k):
    return k if isinstance(k, str) else k[0]


class Sched:
    ENG = ["pe", "act", "dve", "pool", "sp"]
    NEAR = 10 ** 9

    def __init__(self):
        self.q = {e: [] for e in self.ENG}
        self.lastw = {}
        self.readers = {}
        self.alias = {}
        self.touch = {}

    def op(self, eng, fn, r=(), w=(), dma=False):
        w = list(w) + [k for k in r if _buf(k) == "PS"]
        r = [k for k in r if _buf(k) != "PS"]
        idx = len(self.q[eng])
        deps = set()
        for k in r:
            lw = self.lastw.get(k)
            if lw is not None:
                deps.add(lw)
        for k in w:
            lw = self.lastw.get(k)
            if lw is not None:
                deps.add(lw)
            for rd in self.readers.get(k, ()):
                deps.add(rd)
        bufs = set(_buf(k) for k in r) | set(_buf(k) for k in w)
        for b in bufs:
            for a in self.alias.get(b, ()):
                t = self.touch.get(a)
                if t:
                    for e2, i2 in t["c"].items():
                        deps.add((e2, i2))
                    for d in t["d"]:
                        deps.add(d)
        rec = Rec(eng, fn, deps, dma)
        self.q[eng].append(rec)
        me = (eng, idx)
        for k in w:
            self.lastw[k] = me
            self.readers[k] = []
        for k in r:
            self.readers.setdefault(k, []).append(me)
        for b in bufs:
            t = self.touch.setdefault(b, {"c": {}, "d": set()})
            if dma:
                t["d"].add(me)
            else:
                t["c"][eng] = idx
        return rec

    def emit(self, nc, ndsem=24):
        q = self.q
        for e in self.ENG:
            for idx, rec in enumerate(q[e]):
                nd = set()
                best = {}
                for (pe_, pi) in rec.deps:
                    prod = q[pe_][pi]
                    if prod.is_dma:
                        nd.add((pe_, pi))
                        continue
                    if pe_ == e:
                        if e == "pe":
                            continue
                        if idx - pi > self.NEAR:
                            continue
                    if pe_ not in best or best[pe_] < pi:
                        best[pe_] = pi
                for pe_, pi in best.items():
                    nd.add((pe_, pi))
                rec.deps = nd
                for (pe_, pi) in nd:
                    q[pe_][pi].signal = True
        with ExitStack() as es:
            sems = {e: es.enter_context(nc.semaphore("s_" + e)) for e in self.ENG}
            dsems = [es.enter_context(nc.semaphore("d%d" % i)) for i in range(ndsem)]
            for e in self.ENG:
                c = 0
                for rec in q[e]:
                    if rec.is_dma:
                        continue
                    if rec.signal:
                        c += 1
                        rec.semval = c
            dcount = [0] * ndsem
            dlast = [None] * ndsem
            di = 0
            for e in self.ENG:
                for rec in q[e]:
                    if rec.is_dma:
                        s = di % ndsem
                        di += 1
                        dcount[s] += 1
                        rec.dsem = s
                        rec.dval = 16 * dcount[s]
                        rec.prev_dma = dlast[s]
                        dlast[s] = rec
            block = es.enter_context(nc.Block())

            def make(e):
                def body(engine):
                    waited = {}
                    for rec in q[e]:
                        waits = []
                        for (pe_, pi) in rec.deps:
                            prod = q[pe_][pi]
                            if prod.is_dma:
                                waits.append(("d", prod.dsem, prod.dval))
                            else:
                                waits.append(("c", pe_, prod.semval))
                        if rec.is_dma and rec.prev_dma is not None:
                            waits.append(("d", rec.prev_dma.dsem, rec.prev_dma.dval))
                        for (kind, s, v) in waits:
                            key = (kind, s)
                            if waited.get(key, 0) >= v:
                                continue
                            waited[key] = v
                            engine.wait_ge(dsems[s] if kind == "d" else sems[s], v)
                        ins = rec.fn(engine)
                        if rec.is_dma:
                            ins.then_inc(dsems[rec.dsem], 16)
                        elif rec.signal:
                            ins.then_inc(sems[e], 1)
                return body

            block.tensor(make("pe"))
            block.scalar(make("act"))
            block.vector(make("dve"))
            block.gpsimd(make("pool"))
            block.sync(make("sp"))


class Arena:
    def __init__(self, S, g, size):
        self.S = S
        self.g = g
        self.size = size
        self.live = {}
        self.dead = []
        self.peak = 0

    def alloc(self, name, dt, shape, top=False):
        esz = 4 if dt == F32 else 2
        n = int(np.prod(shape[1:])) * esz
        nb = (n + 63) // 64 * 64
        ivs = sorted(self.live.values())
        gaps = []
        cur = 0
        for (o, s) in ivs:
            if o > cur:
                gaps.append((cur, o))
            cur = max(cur, o + s)
        if cur < self.size:
            gaps.append((cur, self.size))
        fit = [(a, b) for (a, b) in gaps if b - a >= nb]
        assert fit, ("arena overflow", name, nb, gaps, sorted((v, k) for k, v in self.live.items()))
        if top:
            a, b = fit[-1]
            off = b - nb
        else:
            off = fit[0][0]
        assert off + nb <= self.size, ("arena overflow", name, off, nb, sorted((v, k) for k, v in self.live.items()))
        assert name not in self.live and name not in self.S.touch, name
        self.live[name] = (off, nb)
        self.peak = max(self.peak, off + nb)
        self.S.alias[name] = [nm for (o, s, nm) in self.dead if o < off + nb and off < o + s]
        self.dead = [(o, s, nm) for (o, s, nm) in self.dead if not (off <= o and o + s <= off + nb)]
        v = self.g[0:shape[0], off:off + n].bitcast(dt)
        if len(shape) == 3:
            v = v.rearrange("p (a b) -> p a b", a=shape[1])
        elif len(shape) == 4:
            v = v.rearrange("p (a b c) -> p a b c", a=shape[1], b=shape[2])
        return v

    def free(self, *names):
        for name in names:
            off, s = self.live.pop(name)
            self.dead.append((off, s, name))


def build(dbg=None):
    nc = bass.Bass("TRN2", target_bir_lowering=False)

    def din(name, shape):
        return nc.dram_tensor(name, list(shape), F32, kind="ExternalInput").ap()

    x_d = din("x", [2048, 1024])
    mem_d = din("mem", [256, 1024])
    w_in_d = din("w_in", [1024, 1280])
    w_out_d = din("w_out", [1024, 1024])
    glu_w_d = din("glu_w", [512, 512])
    wq_d = din("xa_wq", [1024, 1024])
    wk_d = din("xa_wk", [1024, 1024])
    wv_d = din("xa_wv", [1024, 1024])
    wo_d = din("xa_wo", [1024, 1024])
    wr_d = din("wr", [1024, 20])
    wg_d = din("wg", [16, 1024, 512])
    wu_d = din("wu", [16, 1024, 512])
    wd_d = din("wd", [16, 512, 1024])
    vecs_d = din("vecs", [48, 128])
    gfin_d = din("gfin", [1024])
    gq_d = din("gq", [64])
    gk_d = din("gk", [64])
    rb_d = din("rb", [20])
    g64_d = din("g64", [64, 8])
    s5p_d = din("s5p", [96, 128])
    bre_d = din("b_re", [2, 32, 64, 16])
    bim_d = din("b_im", [2, 32, 64, 16])
    cre_d = din("c_re", [2, 32, 16, 64])
    cim_d = din("c_im", [2, 32, 16, 64])
    ident_d = din("ident", [128, 128])
    rope_d = din("rope", [2, 128, 16, 64])
    masks_d = din("masks", [2, 128, 2, 128])
    masksB_d = din("masksB", [128, 4, 128])
    out_d = nc.dram_tensor("out", [2048, 1024], F32, kind="ExternalOutput").ap()
    dbg_d = None

    S = Sched()
    es = ExitStack()
    GSZ = 207 * 1024
    G = es.enter_context(nc.sbuf_tensor("G", [128, GSZ], U8))
    PS = es.enter_context(nc.psum_tensor("PS", [128, 8, 512], F32))
    A = Arena(S, G, GSZ)

    def psk(b):
        return ("PS", b)

    def psbf(b):
        return PS[:, b, :].bitcast(BF16)


    def dbg_finish(items):
        tot = sum(int(ap.shape[1]) for ap, _ in items)
        dd = nc.dram_tensor("dbg", [128, tot], F32, kind="ExternalOutput").ap()
        off = 0
        oks = []
        for i, (ap, keys) in enumerate(items):
            n = int(ap.shape[1])
            if ap.dtype == F32:
                S.op("sp", lambda e, ap=ap, off=off, n=n: e.dma_start(out=dd[:, off:off + n], in_=ap), r=keys, w=["dbgo%d" % i], dma=True)
            else:
                tmp = A.alloc("dbgtmp%d" % i, F32, [128, n])
                S.op("dve", lambda e, tmp=tmp, ap=ap: e.tensor_copy(out=tmp, in_=ap), r=keys, w=["dbgtmp%d" % i])
                S.op("sp", lambda e, tmp=tmp, off=off, n=n: e.dma_start(out=dd[:, off:off + n], in_=tmp), r=["dbgtmp%d" % i], w=["dbgo%d" % i], dma=True)
            oks.append("dbgo%d" % i)
            off += n
        S.op("sp", lambda e: e.nop(), r=oks)
        S.emit(nc)
        es.close()
        return nc

    idf = A.alloc("idf", F32, [128, 128])
    idb = A.alloc("idb", BF16, [128, 128])
    onesb = A.alloc("onesb", BF16, [128, 128])
    gT = A.alloc("gT", F32, [128, 48])
    rbt = A.alloc("rbt", F32, [128, 20])
    gq8 = A.alloc("gq8", F32, [128, 64])
    gkt = A.alloc("gkt", F32, [128, 64])
    X = A.alloc("X", F32, [128, 16, 1024], top=True)
    x_v = x_d.rearrange("(n p) d -> p n d", p=128)
    out_v = out_d.rearrange("(n p) d -> p n d", p=128)

    S.op("sp", lambda e: e.dma_start(out=idf, in_=ident_d), w=["idf"], dma=True)
    for i in range(4):
        S.op("sp", lambda e, i=i: e.dma_start(out=X[:, 4 * i:4 * i + 4, :], in_=x_v[:, 4 * i:4 * i + 4, :]),
             w=[("X", t) for t in range(4 * i, 4 * i + 4)], dma=True)
    vecs_sb = A.alloc("vecs_sb", F32, [128, 128])
    S.op("sp", lambda e: e.dma_start(out=vecs_sb[0:48, :], in_=vecs_d), w=["vecs_sb"], dma=True)
    S.op("sp", lambda e: e.dma_start(out=rbt, in_=rb_d.partition_broadcast(128)), w=["rbt"], dma=True)
    S.op("sp", lambda e: e.dma_start(out=gq8, in_=gq_d.partition_broadcast(128)), w=["gq8"], dma=True)
    S.op("sp", lambda e: e.dma_start(out=gkt, in_=gk_d.partition_broadcast(128)), w=["gkt"], dma=True)
    S.op("dve", lambda e: e.tensor_copy(out=idb, in_=idf), r=["idf"], w=["idb"])
    S.op("pool", lambda e: e.memset(onesb, 1.0), w=["onesb"])
    S.op("pe", lambda e: e.transpose(out=PS[:, 7, 0:48], in_=vecs_sb[0:48, :], identity=idf[0:48, 0:48]),
         r=["vecs_sb", "idf"], w=[psk(7)])
    S.op("dve", lambda e: e.tensor_copy(out=gT, in_=PS[:, 7, 0:48]), r=[psk(7)], w=["gT"])
    S.op("dve", lambda e: e.tensor_scalar(out=gq8, in0=gq8, scalar1=0.125, scalar2=None, op0=ALU.mult), r=["gq8"], w=["gq8"])
    A.free("vecs_sb")

    if dbg == "s0":
        return dbg_finish([(gT, ["gT"]), (gq8, ["gq8"]), (gkt, ["gkt"]), (rbt, ["rbt"]), (X[:, 3, :], [("X", 3)])])
    wstate = {"st": 0, "wb": 0, "nst": 0, "nwb": 0, "ST": None, "WB": None}

    def w_open(nst, nwb):
        wstate["ST"] = [A.alloc("ST%d_%d" % (i, wstate["nst"] + 1000 * nst), F32, [128, 4096]) for i in range(nst)]
        wstate["STn"] = ["ST%d_%d" % (i, wstate["nst"] + 1000 * nst) for i in range(nst)]
        wstate["WB"] = [A.alloc("WB%d_%d" % (i, wstate["nwb"] + 1000 * nwb), BF16, [128, 4096]) for i in range(nwb)]
        wstate["WBn"] = ["WB%d_%d" % (i, wstate["nwb"] + 1000 * nwb) for i in range(nwb)]
        wstate["nst"] += 1
        wstate["nwb"] += 1
        wstate["st"] = 0
        wstate["wb"] = 0

    def w_close():
        A.free(*wstate["STn"])
        A.free(*wstate["WBn"])

    def load_w(src, a, b, gcol=None, eng="pool"):
        si = wstate["st"] % len(wstate["ST"])
        wi = wstate["wb"] % len(wstate["WB"])
        wstate["st"] += 1
        wstate["wb"] += 1
        st = wstate["ST"][si][:, 0:a * b].rearrange("p (a b) -> p a b", a=a)
        wb = wstate["WB"][wi][:, 0:a * b].rearrange("p (a b) -> p a b", a=a)
        sk = wstate["STn"][si]
        wk = wstate["WBn"][wi]
        S.op("sp", lambda e: e.dma_start(out=st, in_=src), w=[sk], dma=True)
        if gcol is None:
            S.op(eng, lambda e: e.tensor_scalar(out=wb, in0=st, scalar1=1.0, scalar2=0.0, op0=ALU.mult, op1=ALU.add), r=[sk], w=[wk])
        else:
            S.op(eng, lambda e: e.tensor_tensor(out=wb, in0=st, in1=gT[:, gcol:gcol + a].unsqueeze(2).broadcast_to([128, a, b]), op=ALU.mult),
                 r=[sk, "gT"], w=[wk])
        return wb, wk

    cnt = {"n": 0}

    def alt():
        cnt["n"] += 1
        return "act" if cnt["n"] % 2 else "dve"

    def copy_op(eng, out, in_, r, w):
        if eng == "act":
            S.op("act", lambda e: e.copy(out=out, in_=in_), r=r, w=w)
        else:
            S.op(eng, lambda e: e.tensor_copy(out=out, in_=in_), r=r, w=w)

    def rms_stats(src_tiles, ntile, width, name, group=4):
        ss = A.alloc(name + "_ss", F32, [128, ntile])
        rstd = A.alloc(name, F32, [128, ntile])
        junk = A.alloc(name + "_junk", BF16, [128, width])
        for t0 in range(0, ntile, group):
            for t in range(t0, min(ntile, t0 + group)):
                ap, keys = src_tiles(t)
                S.op("act", lambda e, ap=ap, t=t: e.activation(out=junk, in_=ap, func=AF.Square, accum_out=ss[:, t:t + 1]),
                     r=keys, w=[name + "_junk", (name + "_ss", t)])
            t1 = min(ntile, t0 + group)
            S.op("act", lambda e, t0=t0, t1=t1: e.activation(out=rstd[:, t0:t1], in_=ss[:, t0:t1], func=AF.Sqrt, scale=1.0 / width, bias=EPS),
                 r=[(name + "_ss", t) for t in range(t0, t1)], w=[(name, t) for t in range(t0, t1)])
            S.op("dve", lambda e, t0=t0, t1=t1: e.reciprocal(out=rstd[:, t0:t1], in_=rstd[:, t0:t1]),
                 r=[(name, t) for t in range(t0, t1)], w=[(name, t) for t in range(t0, t1)])
        A.free(name + "_ss", name + "_junk")
        return rstd

    def norm_transpose(src_tiles, ntile, nk, rstd, rname, dst, dname, banks, tmpname):
        hn = [A.alloc("%s_hn%d" % (tmpname, i), BF16, [128, nk * 128]) for i in range(2)]
        for t in range(ntile):
            ap, keys = src_tiles(t)
            h = hn[t % 2]
            hk = "%s_hn%d" % (tmpname, t % 2)
            eng = alt()
            if eng == "act":
                S.op("act", lambda e, ap=ap, h=h, t=t: e.activation(out=h, in_=ap, func=AF.Identity, scale=rstd[:, t:t + 1]),
                     r=keys + [(rname, t)], w=[hk])
            else:
                S.op("dve", lambda e, ap=ap, h=h, t=t: e.tensor_scalar(out=h, in0=ap, scalar1=rstd[:, t:t + 1], scalar2=None, op0=ALU.mult),
                     r=keys + [(rname, t)], w=[hk])
            b = banks[t % len(banks)]
            pv = psbf(b)
            for k in range(nk):
                S.op("pe", lambda e, k=k, h=h, pv=pv: e.transpose(out=pv[:, k * 128:(k + 1) * 128], in_=h[:, k * 128:(k + 1) * 128], identity=idb),
                     r=[hk, "idb"], w=[psk(b)])
            copy_op("dve", dst[:, 0:nk, t * 128:(t + 1) * 128], pv[:, 0:nk * 128].rearrange("p (k c) -> p k c", k=nk), [psk(b)], [(dname, t)])
        A.free("%s_hn0" % tmpname, "%s_hn1" % tmpname)

    def xtile(t):
        return X[:, t, :], [("X", t)]

    HT = A.alloc("HT", BF16, [128, 8, 2048], top=True)
    w_open(2, 3)
    wqb, wqk = load_w(w_in_d[:, 0:512].rearrange("(k p) f -> p k f", p=128), 8, 512, gcol=0)
    wkvb, wkvk = load_w(w_in_d[:, 512:768].rearrange("(k p) f -> p k f", p=128), 8, 256, gcol=0)
    rstd1 = rms_stats(xtile, 16, 1024, "rstd1")
    norm_transpose(xtile, 16, 8, rstd1, "rstd1", HT, "HT", [0, 1], "n1")
    A.free("rstd1")
    if dbg == "s1":
        return dbg_finish([(HT[:, 2, 0:1024], [("HT", t) for t in range(8)]), (wqb[:, 3, :], [wqk])])
    wub, wuk = load_w(w_in_d[:, 768:1280].rearrange("(k p) f -> p k f", p=128), 8, 512, gcol=0)

    ropeC = A.alloc("ropeC", F32, [128, 16, 64])
    ropeS = A.alloc("ropeS", F32, [128, 16, 64])
    S.op("sp", lambda e: e.dma_start(out=ropeC, in_=rope_d[0]), w=["ropeC"], dma=True)
    S.op("sp", lambda e: e.dma_start(out=ropeS, in_=rope_d[1]), w=["ropeS"], dma=True)
    QT = A.alloc("QT", BF16, [128, 4, 2048])
    KT = A.alloc("KT", BF16, [128, 2, 2048])
    S.op("pool", lambda e: e.memset(KT, 0.0), w=["KT"])
    VA = A.alloc("VA", BF16, [128, 16, 2, 128])
    S.op("pool", lambda e: e.memset(VA, 0.0), w=["VA"])
    S.op("pool", lambda e: e.memset(VA[:, :, :, 64:65], 1.0), r=["VA"], w=["VA"])
    A.free(*wstate["STn"])
    wstate["STn"] = []
    sq = [A.alloc("qk_sq%d" % i, F32, [128, 10, 64]) for i in range(2)]
    n0s = [A.alloc("qk_n0%d" % i, F32, [128, 10, 64]) for i in range(2)]
    n1s = [A.alloc("qk_n1%d" % i, F32, [128, 10, 64]) for i in range(2)]
    qbs = [A.alloc("qk_b%d" % i, F32, [128, 10, 64]) for i in range(2)]
    qsss = [A.alloc("qk_ss%d" % i, F32, [128, 10]) for i in range(2)]
    qtm = [A.alloc("qk_tm%d" % i, BF16, [128, 5, 2, 64]) for i in range(2)]

    def st2_A(t):
        p = t % 2
        bq, bkv = 2 + p, 4 + p
        for k in range(8):
            S.op("pe", lambda e, k=k: e.matmul(PS[:, bq, :], lhsT=HT[:, k, t * 128:(t + 1) * 128], rhs=wqb[:, k, :], start=(k == 0), stop=(k == 7)),
                 r=[("HT", t), wqk], w=[psk(bq)])
        for k in range(8):
            S.op("pe", lambda e, k=k: e.matmul(PS[:, bkv, 0:256], lhsT=HT[:, k, t * 128:(t + 1) * 128], rhs=wkvb[:, k, :], start=(k == 0), stop=(k == 7)),
                 r=[("HT", t), wkvk], w=[psk(bkv)])
        qv = PS[:, bq, :].rearrange("p (h d) -> p h d", h=8)
        kv_ = PS[:, bkv, 0:128].rearrange("p (h d) -> p h d", h=2)
        sqk, ssk, n0k = "qk_sq%d" % p, "qk_ss%d" % p, "qk_n0%d" % p
        S.op("act", lambda e: e.activation(out=sq[p][:, 0:8, :], in_=qv, func=AF.Square), r=[psk(bq)], w=[sqk])
        S.op("act", lambda e: e.activation(out=sq[p][:, 8:10, :], in_=kv_, func=AF.Square), r=[psk(bkv)], w=[sqk])
        S.op("dve", lambda e: e.tensor_reduce(out=qsss[p], in_=sq[p], axis=AX.X, op=ALU.add), r=[sqk], w=[ssk])
        S.op("act", lambda e: e.activation(out=qsss[p], in_=qsss[p], func=AF.Sqrt, scale=1.0 / 64, bias=EPS), r=[ssk], w=[ssk])
        S.op("dve", lambda e: e.reciprocal(out=qsss[p], in_=qsss[p]), r=[ssk], w=[ssk])
        S.op("dve", lambda e: e.tensor_tensor(out=n0s[p][:, 0:8, :], in0=qv, in1=qsss[p][:, 0:8].unsqueeze(2).broadcast_to([128, 8, 64]), op=ALU.mult),
             r=[psk(bq), ssk], w=[n0k])
        S.op("dve", lambda e: e.tensor_tensor(out=n0s[p][:, 8:10, :], in0=kv_, in1=qsss[p][:, 8:10].unsqueeze(2).broadcast_to([128, 2, 64]), op=ALU.mult),
             r=[psk(bkv), ssk], w=[n0k])
        S.op("act", lambda e: e.copy(out=VA[:, t, :, 0:64], in_=PS[:, bkv, 128:256].rearrange("p (h d) -> p h d", h=2)),
             r=[psk(bkv), "VA"], w=[("VA", t)])

    def st2_B(t):
        p = t % 2
        n0, n1, qb_ = n0s[p], n1s[p], qbs[p]
        n0k, n1k, qbk, tmk = "qk_n0%d" % p, "qk_n1%d" % p, "qk_b%d" % p, "qk_tm%d" % p
        tm = qtm[p]
        S.op("pool", lambda e: e.tensor_tensor(out=n1[:, 0:8, :], in0=n0[:, 0:8, :], in1=gq8.unsqueeze(1).broadcast_to([128, 8, 64]), op=ALU.mult),
             r=[n0k, "gq8"], w=[n1k])
        S.op("pool", lambda e: e.tensor_tensor(out=n1[:, 8:10, :], in0=n0[:, 8:10, :], in1=gkt.unsqueeze(1).broadcast_to([128, 2, 64]), op=ALU.mult),
             r=[n0k, "gkt"], w=[n1k])
        S.op("dve", lambda e: e.tensor_tensor(out=n0, in0=n1, in1=ropeC[:, t, :].unsqueeze(1).broadcast_to([128, 10, 64]), op=ALU.mult),
             r=[n1k, "ropeC"], w=[n0k])
        n1v = n1.rearrange("p h (b f d) -> p h b f d", b=2, f=2)
        qbv = qb_.rearrange("p h (b f d) -> p h b f d", b=2, f=2)
        for f in range(2):
            S.op("pool", lambda e, f=f: e.tensor_tensor(
                out=qbv[:, :, :, f, :], in0=n1v[:, :, :, 1 - f, :],
                in1=ropeS[:, t, :].rearrange("p (b f d) -> p b f d", b=2, f=2)[:, :, f, :].unsqueeze(1).broadcast_to([128, 10, 2, 16]),
                op=ALU.mult), r=[n1k, "ropeS"], w=[(qbk, f)])
        S.op("dve", lambda e: e.tensor_tensor(out=tm[:, 0:4, :, :], in0=n0[:, 0:8, :].rearrange("p (kv j) d -> p j kv d", kv=2),
                                              in1=qb_[:, 0:8, :].rearrange("p (kv j) d -> p j kv d", kv=2), op=ALU.add),
             r=[n0k, (qbk, 0), (qbk, 1)], w=[(tmk, 0)])
        S.op("dve", lambda e: e.tensor_tensor(out=tm[:, 4, :, :], in0=n0[:, 8:10, :], in1=qb_[:, 8:10, :], op=ALU.add),
             r=[n0k, (qbk, 0), (qbk, 1)], w=[(tmk, 1)])

    def st2_C(t):
        p = t % 2
        tm, tmk = qtm[p], "qk_tm%d" % p
        bt = 6 + p
        pv = psbf(bt)
        for j in range(5):
            S.op("pe", lambda e, j=j: e.transpose(out=pv[:, j * 128:(j + 1) * 128], in_=tm[:, j, :, :].rearrange("p a d -> p (a d)"), identity=idb),
                 r=[(tmk, 0), (tmk, 1), "idb"], w=[psk(bt)])
        copy_op("dve", QT[:, :, t * 128:(t + 1) * 128], pv[:, 0:512].rearrange("p (k c) -> p k c", k=4), [psk(bt)], [("QT", t)])
        copy_op("dve", KT[0:64, 0, t * 128:(t + 1) * 128], pv[0:64, 512:640], [psk(bt), "KT"], [("KT", t, 0)])
        copy_op("dve", KT[64:128, 1, t * 128:(t + 1) * 128], pv[64:128, 512:640], [psk(bt), "KT"], [("KT", t, 1)])

    st2_A(0)
    st2_A(1)
    st2_B(0)
    for t in range(16):
        if t + 2 < 16:
            st2_A(t + 2)
        if t + 1 < 16:
            st2_B(t + 1)
        st2_C(t)
    A.free("ropeC", "ropeS", "qk_sq0", "qk_sq1", "qk_n00", "qk_n01", "qk_n10", "qk_n11", "qk_b0", "qk_b1", "qk_ss0", "qk_ss1", "qk_tm0", "qk_tm1")

    UT = A.alloc("UT", BF16, [128, 4, 8, 256], top=True)
    for fc in range(4):
        for tc in range(4):
            b = (fc * 4 + tc) % 2
            for k in range(8):
                S.op("pe", lambda e, fc=fc, tc=tc, k=k, b=b: e.matmul(PS[:, b, :], lhsT=wub[:, k, fc * 128:(fc + 1) * 128], rhs=HT[:, k, tc * 512:(tc + 1) * 512],
                                                                      start=(k == 0), stop=(k == 7)),
                     r=[("HT", t) for t in range(4 * tc, 4 * tc + 4)] + [wuk], w=[psk(b)])
            copy_op(alt(), UT[:, fc, :, tc * 64:(tc + 1) * 64], PS[:, b, :].rearrange("p (b j) -> p j b", j=8), [psk(b)], [("UT", fc, tc)])
    w_close()
    A.free("HT")

    if dbg == "stage2":
        dbg_d = nc.dram_tensor("dbg", [128, 4 * 2048 + 2048 + 16 * 130 + 4 * 2048], F32, kind="ExternalOutput").ap()
        dt_ = A.alloc("dbgt", F32, [128, 4 * 2048])
        S.op("dve", lambda e: e.tensor_copy(out=dt_, in_=QT.rearrange("p a b -> p (a b)")), r=[("QT", t) for t in range(16)], w=["dbgt"])
        S.op("sp", lambda e: e.dma_start(out=dbg_d[:, 0:8192], in_=dt_), r=["dbgt"], w=["o1"], dma=True)
        S.op("dve", lambda e: e.tensor_tensor(out=dt_[:, 0:2048], in0=KT[:, 0, :], in1=KT[:, 1, :], op=ALU.add), r=[("KT", t, kk) for t in range(16) for kk in range(2)] + ["o1"], w=["dbgt"])
        S.op("sp", lambda e: e.dma_start(out=dbg_d[:, 8192:8192 + 2048], in_=dt_[:, 0:2048]), r=["dbgt"], w=["o2"], dma=True)
        S.op("dve", lambda e: e.tensor_copy(out=dt_[:, 0:2080].rearrange("p (a c) -> p a c", c=65), in_=VA.rearrange("p a b c -> p (a b) c")[:, :, 0:65]), r=[("VA", t) for t in range(16)] + ["o2"], w=["dbgt"])
        S.op("sp", lambda e: e.dma_start(out=dbg_d[:, 10240:10240 + 2080], in_=dt_[:, 0:2080]), r=["dbgt"], w=["o3"], dma=True)
        S.op("dve", lambda e: e.tensor_copy(out=dt_, in_=UT.rearrange("p a j b -> p (a j b)")), r=[("UT", a, b) for a in range(4) for b in range(4)] + ["o3"], w=["dbgt"])
        S.op("sp", lambda e: e.dma_start(out=dbg_d[:, 12320:12320 + 8192], in_=dt_), r=["dbgt"], w=["o4"], dma=True)
        S.op("sp", lambda e: e.nop(), r=["o1", "o2", "o3", "o4"])
        S.emit(nc)
        es.close()
        return nc

    YA = A.alloc("YA", BF16, [128, 8, 2048], top=True)
    S.op("pool", lambda e: e.memset(YA[64:128, :, :], 0.0), w=["YA"])
    PT = [A.alloc("PT%d" % i, BF16, [128, 512]) for i in range(5)]
    SBANK = [0, 1, 7, 5]
    SR = [A.alloc("SR%d" % i, F32, [128, 512]) for i in range(2)]
    RI = [A.alloc("RI%d" % i, F32, [128, 512]) for i in range(2)]
    onesf = A.alloc("onesf", F32, [128, 64])
    S.op("pool", lambda e: e.memset(onesf, 0.0), w=["onesf"])
    S.op("pool", lambda e: e.memset(onesf[64:65, :], 1.0), r=["onesf"], w=["onesf"])
    for i_ in range(2):
        S.op("pool", lambda e, i_=i_: e.memset(SR[i_], 0.0), w=["SR%d" % i_])
    its = [(qc, j, kv, kt) for qc in range(4) for j in range(4) for kv in range(2) for kt in range(16)]
    deferred = []
    g64 = A.alloc("g64", F32, [128, 8])
    S.op("sp", lambda e: e.dma_start(out=g64[0:64, :], in_=g64_d), w=["g64"], dma=True)
    wst_a = A.alloc("wst_a", F32, [128, 8, 512])
    woa = [A.alloc("woa%d" % i, BF16, [128, 8, 512]) for i in range(2)]
    for dc in range(2):
        S.op("pool", lambda e, dc=dc: e.memset(woa[dc][64:128, :, :], 0.0), w=["woa%d" % dc])
        S.op("sp", lambda e, dc=dc: e.dma_start(out=wst_a[0:64, :, :], in_=w_out_d[0:512, dc * 512:(dc + 1) * 512].rearrange("(h p) f -> p h f", p=64)),
             w=["wst_a"], dma=True)
        S.op("pool", lambda e, dc=dc: e.tensor_tensor(out=woa[dc][0:64, :, :], in0=wst_a[0:64, :, :], in1=g64[0:64, :].unsqueeze(2).broadcast_to([64, 8, 512]), op=ALU.mult),
             r=["wst_a", "g64"], w=["woa%d" % dc])

    def att_wout(qc):
        for t in range(4 * qc, 4 * qc + 4):
            for dc in range(2):
                b = 6
                for h in range(8):
                    S.op("pe", lambda e, t=t, dc=dc, h=h, b=b: e.matmul(PS[:, b, :], lhsT=YA[:, h, t * 128:(t + 1) * 128], rhs=woa[dc][:, h, :], start=(h == 0), stop=(h == 7)),
                         r=[("YA", qc, h), "YA", "woa%d" % dc], w=[psk(b)])
                S.op("dve", lambda e, t=t, dc=dc, b=b: e.tensor_tensor(out=X[:, t, dc * 512:(dc + 1) * 512], in0=PS[:, b, :], in1=X[:, t, dc * 512:(dc + 1) * 512], op=ALU.add),
                     r=[psk(b), ("X", t)], w=[("X", t)])

    def att_scores(i):
        qc, j, kv, kt = its[i]
        b = SBANK[i % 4]
        S.op("pe", lambda e: e.matmul(PS[:, b, :], lhsT=KT[:, kv, kt * 128:(kt + 1) * 128],
                                      rhs=QT[:, j, qc * 512:(qc + 1) * 512], start=True, stop=True),
             r=[("KT", kt, kv)] + [("QT", t) for t in range(4 * qc, 4 * qc + 4)], w=[psk(b)])
        S.op("act", lambda e: e.activation(out=PT[i % 5], in_=PS[:, b, :], func=AF.Exp), r=[psk(b)], w=["PT%d" % (i % 5)])

    def att_finish(hi, qc, h, ob):
        sr, srk = SR[hi % 2], "SR%d" % (hi % 2)
        ri, rik = RI[hi % 2], "RI%d" % (hi % 2)
        nb = 4
        S.op("dve", lambda e: e.tensor_copy(out=sr[64:65, :], in_=PS[64:65, ob, :]), r=[psk(ob)], w=[srk])
        S.op("pe", lambda e: e.matmul(PS[0:64, nb, :], lhsT=onesf, rhs=sr, start=True, stop=True), r=["onesf", srk], w=[psk(nb)])
        S.op("dve", lambda e: e.reciprocal(out=ri[0:64, :], in_=PS[0:64, nb, :]), r=[psk(nb)], w=[rik])
        S.op("dve", lambda e: e.tensor_tensor(out=YA[0:64, h, qc * 512:(qc + 1) * 512], in0=PS[0:64, ob, :], in1=ri[0:64, :], op=ALU.mult),
             r=[psk(ob), rik], w=[("YA", qc, h)])

    sqa = A.alloc("sqa", BF16, [128, 8, 512])

    def att_norm(qc):
        yk = [("YA", qc, h) for h in range(8)]
        ri, rik = RI[0], "RI0"
        S.op("act", lambda e: e.activation(out=sqa[0:64, :, :], in_=YA[0:64, :, qc * 512:(qc + 1) * 512], func=AF.Square), r=yk, w=["sqa"])
        for h in range(8):
            S.op("pe", lambda e, h=h: e.matmul(PS[0:64, 6, :], lhsT=onesb[0:64, 0:64], rhs=sqa[0:64, h, :], start=(h == 0), stop=(h == 7)), r=["onesb", "sqa"], w=[psk(6)])
        S.op("act", lambda e: e.activation(out=ri[0:64, :], in_=PS[0:64, 6, :], func=AF.Sqrt, scale=1.0 / 512, bias=EPS), r=[psk(6)], w=[rik])
        S.op("dve", lambda e: e.reciprocal(out=ri[0:64, :], in_=ri[0:64, :]), r=[rik], w=[rik])
        S.op("dve", lambda e: e.tensor_tensor(out=YA[0:64, :, qc * 512:(qc + 1) * 512], in0=YA[0:64, :, qc * 512:(qc + 1) * 512],
                                              in1=ri[0:64, :].unsqueeze(1).broadcast_to([64, 8, 512]), op=ALU.mult), r=yk + [rik], w=yk)

    def att_pv(i):
        qc, j, kv, kt = its[i]
        hi = i // 16
        ob = 2 + (hi % 2)
        S.op("pe", lambda e: e.matmul(PS[:, ob, :], lhsT=VA[:, kt, kv, :], rhs=PT[i % 5], start=(kt == 0), stop=(kt == 15)),
             r=["PT%d" % (i % 5), ("VA", kt)], w=[psk(ob)])
        if kt == 15:
            h = kv * 4 + j
            deferred.append((i + 3, lambda: att_finish(hi, qc, h, ob)))
            if h == 7:
                deferred.append((i + 4, lambda: att_norm(qc)))
                deferred.append((i + 8, lambda: att_wout(qc)))

    def run_deferred(i):
        while deferred and deferred[0][0] <= i:
            deferred.pop(0)[1]()

    att_scores(0)
    att_scores(1)
    att_scores(2)
    att_scores(3)
    for i in range(len(its)):
        att_pv(i)
        if i + 4 < len(its):
            att_scores(i + 4)
        run_deferred(i)
    run_deferred(10 ** 9)
    A.free("QT", "KT", "VA", "PT0", "PT1", "PT2", "PT3", "PT4", "SR0", "SR1", "RI0", "RI1", "sqa", "onesf")

    if dbg == "att":
        A.free("wst_a", "woa0", "woa1")
        return dbg_finish([(YA[:, h, :], [("YA", qc, h) for qc in range(4)]) for h in range(8)])
    A.free("YA", "g64", "wst_a", "woa0", "woa1")
    s5raw = A.alloc("s5raw", F32, [128, 128])
    S.op("sp", lambda e: e.dma_start(out=s5raw[0:96, :], in_=s5p_d), w=["s5raw"], dma=True)
    S.op("pe", lambda e: e.transpose(out=PS[:, 7, 0:96], in_=s5raw[0:96, :], identity=idf[0:96, 0:96]), r=["s5raw", "idf"], w=[psk(7)])
    P5 = A.alloc("P5", F32, [128, 24, 32])
    NV = {"i": 3}

    def pv_(i):
        return P5[:, i, :]

    S.op("dve", lambda e: e.tensor_copy(out=P5[:, 0:3, :], in_=PS[:, 7, 0:96].rearrange("p (a b) -> p a b", a=3)), r=[psk(7)], w=["P5"])
    A.free("s5raw")
    LR, LI, LS, DT, MAG, ANG, KK, R1, SN, CS, ABR, ABI, DEN, NR, FR, FI, T0, T1, NABI = range(19)

    def v1(eng, fn):
        S.op(eng, fn, r=["P5"], w=["P5"])

    MAGIC = 12582912.0
    C1 = 6.28125
    C2 = float(2 * np.pi - 6.28125)
    v1("dve", lambda e: e.tensor_scalar(out=pv_(LR), in0=pv_(LR), scalar1=-1e-4, scalar2=None, op0=ALU.min))
    v1("act", lambda e: e.activation(out=pv_(DT), in_=pv_(LS), func=AF.Exp))
    v1("dve", lambda e: e.tensor_tensor(out=pv_(T0), in0=pv_(LR), in1=pv_(DT), op=ALU.mult))
    v1("act", lambda e: e.activation(out=pv_(MAG), in_=pv_(T0), func=AF.Exp))
    v1("dve", lambda e: e.tensor_tensor(out=pv_(ANG), in0=pv_(LI), in1=pv_(DT), op=ALU.mult))

    def sin_of(dst, shift):
        v1("dve", lambda e: e.tensor_scalar(out=pv_(T0), in0=pv_(ANG), scalar1=float(shift), scalar2=None, op0=ALU.add))
        v1("dve", lambda e: e.tensor_scalar(out=pv_(KK), in0=pv_(T0), scalar1=float(1 / (2 * np.pi)), scalar2=MAGIC, op0=ALU.mult, op1=ALU.add))
        v1("dve", lambda e: e.tensor_scalar(out=pv_(T1), in0=pv_(KK), scalar1=-MAGIC, scalar2=None, op0=ALU.add))
        v1("dve", lambda e: e.scalar_tensor_tensor(out=pv_(R1), in0=pv_(T1), scalar=-C1, in1=pv_(T0), op0=ALU.mult, op1=ALU.add))
        v1("dve", lambda e: e.scalar_tensor_tensor(out=pv_(KK), in0=pv_(T1), scalar=-C2, in1=pv_(R1), op0=ALU.mult, op1=ALU.add))
        v1("dve", lambda e: e.tensor_scalar(out=pv_(KK), in0=pv_(KK), scalar1=3.1415925, scalar2=-3.1415925, op0=ALU.min, op1=ALU.max))
        v1("act", lambda e: e.activation(out=pv_(dst), in_=pv_(KK), func=AF.Sin))

    sin_of(SN, 0.0)
    sin_of(CS, np.pi / 2)
    v1("dve", lambda e: e.tensor_tensor(out=pv_(ABR), in0=pv_(MAG), in1=pv_(CS), op=ALU.mult))
    v1("dve", lambda e: e.tensor_tensor(out=pv_(ABI), in0=pv_(MAG), in1=pv_(SN), op=ALU.mult))
    v1("dve", lambda e: e.tensor_scalar(out=pv_(NABI), in0=pv_(ABI), scalar1=-1.0, scalar2=None, op0=ALU.mult))
    v1("dve", lambda e: e.tensor_tensor(out=pv_(DEN), in0=pv_(LR), in1=pv_(LR), op=ALU.mult))
    v1("dve", lambda e: e.tensor_tensor(out=pv_(T0), in0=pv_(LI), in1=pv_(LI), op=ALU.mult))
    v1("dve", lambda e: e.tensor_tensor(out=pv_(DEN), in0=pv_(DEN), in1=pv_(T0), op=ALU.add))
    v1("dve", lambda e: e.reciprocal(out=pv_(DEN), in_=pv_(DEN)))
    v1("dve", lambda e: e.tensor_scalar(out=pv_(NR), in0=pv_(ABR), scalar1=-1.0, scalar2=None, op0=ALU.add))
    v1("dve", lambda e: e.tensor_tensor(out=pv_(T0), in0=pv_(NR), in1=pv_(LR), op=ALU.mult))
    v1("dve", lambda e: e.tensor_tensor(out=pv_(T1), in0=pv_(ABI), in1=pv_(LI), op=ALU.mult))
    v1("dve", lambda e: e.tensor_tensor(out=pv_(T0), in0=pv_(T0), in1=pv_(T1), op=ALU.add))
    v1("dve", lambda e: e.tensor_tensor(out=pv_(FR), in0=pv_(T0), in1=pv_(DEN), op=ALU.mult))
    v1("dve", lambda e: e.tensor_tensor(out=pv_(T0), in0=pv_(ABI), in1=pv_(LR), op=ALU.mult))
    v1("dve", lambda e: e.tensor_tensor(out=pv_(T1), in0=pv_(NR), in1=pv_(LI), op=ALU.mult))
    v1("dve", lambda e: e.tensor_tensor(out=pv_(T0), in0=pv_(T0), in1=pv_(T1), op=ALU.subtract))
    v1("dve", lambda e: e.tensor_tensor(out=pv_(FI), in0=pv_(T0), in1=pv_(DEN), op=ALU.mult))

    A16R, A16I = 19, 20
    S.op("dve", lambda e: e.tensor_copy(out=pv_(A16R), in_=pv_(ABR)), r=["P5"], w=["P5"])
    S.op("dve", lambda e: e.tensor_copy(out=pv_(A16I), in_=pv_(ABI)), r=["P5"], w=["P5"])
    for _ in range(3):
        v1("dve", lambda e: e.tensor_tensor(out=pv_(T0), in0=pv_(A16R), in1=pv_(A16R), op=ALU.mult))
        v1("dve", lambda e: e.tensor_tensor(out=pv_(T1), in0=pv_(A16I), in1=pv_(A16I), op=ALU.mult))
        v1("dve", lambda e: e.tensor_tensor(out=pv_(KK), in0=pv_(A16R), in1=pv_(A16I), op=ALU.mult))
        v1("dve", lambda e: e.tensor_tensor(out=pv_(A16R), in0=pv_(T0), in1=pv_(T1), op=ALU.subtract))
        v1("dve", lambda e: e.tensor_scalar(out=pv_(A16I), in0=pv_(KK), scalar1=2.0, scalar2=None, op0=ALU.mult))
    PW2R = A.alloc("PW2R", F32, [128, 32, 16])
    PW2I = A.alloc("PW2I", F32, [128, 32, 16])
    PWt = A.alloc("PWt", F32, [128, 2, 32, 8])
    kp2 = ["PW2R", "PW2I", "PWt", "P5"]
    S.op("dve", lambda e: e.tensor_copy(out=PW2R[:, :, 0], in_=pv_(A16R)), r=kp2, w=kp2)
    S.op("dve", lambda e: e.tensor_copy(out=PW2I[:, :, 0], in_=pv_(A16I)), r=kp2, w=kp2)
    m_ = 1
    while m_ < 16:
        br_ = PW2R[:, :, m_ - 1].unsqueeze(2).broadcast_to([128, 32, m_])
        bi_ = PW2I[:, :, m_ - 1].unsqueeze(2).broadcast_to([128, 32, m_])
        t0_, t1_ = PWt[:, 0, :, 0:m_], PWt[:, 1, :, 0:m_]
        pr_, pi2_ = PW2R[:, :, 0:m_], PW2I[:, :, 0:m_]
        S.op("dve", lambda e, t0_=t0_, pr_=pr_, br_=br_: e.tensor_tensor(out=t0_, in0=pr_, in1=br_, op=ALU.mult), r=kp2, w=kp2)
        S.op("dve", lambda e, t1_=t1_, pi2_=pi2_, bi_=bi_: e.tensor_tensor(out=t1_, in0=pi2_, in1=bi_, op=ALU.mult), r=kp2, w=kp2)
        S.op("dve", lambda e, m_=m_, t0_=t0_, t1_=t1_: e.tensor_tensor(out=PW2R[:, :, m_:2 * m_], in0=t0_, in1=t1_, op=ALU.subtract), r=kp2, w=kp2)
        S.op("dve", lambda e, t0_=t0_, pr_=pr_, bi_=bi_: e.tensor_tensor(out=t0_, in0=pr_, in1=bi_, op=ALU.mult), r=kp2, w=kp2)
        S.op("dve", lambda e, t1_=t1_, pi2_=pi2_, br_=br_: e.tensor_tensor(out=t1_, in0=pi2_, in1=br_, op=ALU.mult), r=kp2, w=kp2)
        S.op("dve", lambda e, m_=m_, t0_=t0_, t1_=t1_: e.tensor_tensor(out=PW2I[:, :, m_:2 * m_], in0=t0_, in1=t1_, op=ALU.add), r=kp2, w=kp2)
        m_ *= 2
    A.free("PWt")

    masksB = A.alloc("masksB", F32, [128, 4, 128])
    S.op("sp", lambda e: e.dma_start(out=masksB, in_=masksB_d), w=["masksB"], dma=True)
    masks = A.alloc("masks", F32, [128, 2, 2, 128])
    S.op("sp", lambda e: e.dma_start(out=masks[:, 0, :, :], in_=masks_d[0]), w=["masks"], dma=True)
    S.op("sp", lambda e: e.dma_start(out=masks[:, 1, :, :], in_=masks_d[1]), w=["masks"], dma=True)
    Bre = A.alloc("Bre", F32, [128, 32, 16])
    Bim = A.alloc("Bim", F32, [128, 32, 16])
    BBr = A.alloc("BBr", F32, [128, 32, 16])
    BBi = A.alloc("BBi", F32, [128, 32, 16])
    Bt = A.alloc("Bt", F32, [128, 32, 16])
    for dd in range(2):
        S.op("sp", lambda e, dd=dd: e.dma_start(out=Bre[:, 16 * dd:16 * dd + 16, :], in_=bre_d[dd].rearrange("(gp gl) p c -> (gl p) gp c", gl=2)),
             w=["Bre"], dma=True)
        S.op("sp", lambda e, dd=dd: e.dma_start(out=Bim[:, 16 * dd:16 * dd + 16, :], in_=bim_d[dd].rearrange("(gp gl) p c -> (gl p) gp c", gl=2)),
             w=["Bim"], dma=True)

    def bc(i):
        return pv_(i).unsqueeze(2).broadcast_to([128, 32, 16])

    S.op("dve", lambda e: e.tensor_tensor(out=BBr, in0=Bre, in1=bc(FR), op=ALU.mult), r=["Bre", "P5"], w=["BBr"])
    S.op("dve", lambda e: e.tensor_tensor(out=Bt, in0=Bim, in1=bc(FI), op=ALU.mult), r=["Bim", "P5"], w=["Bt"])
    S.op("dve", lambda e: e.tensor_tensor(out=BBr, in0=BBr, in1=Bt, op=ALU.subtract), r=["BBr", "Bt"], w=["BBr"])
    S.op("dve", lambda e: e.tensor_tensor(out=BBi, in0=Bim, in1=bc(FR), op=ALU.mult), r=["Bim", "P5"], w=["BBi"])
    S.op("dve", lambda e: e.tensor_tensor(out=Bt, in0=Bre, in1=bc(FI), op=ALU.mult), r=["Bre", "P5", "BBr"], w=["Bt"])
    S.op("dve", lambda e: e.tensor_tensor(out=BBi, in0=BBi, in1=Bt, op=ALU.add), r=["BBi", "Bt"], w=["BBi"])
    A.free("Bre", "Bim", "Bt")
    TB = A.alloc("TB", BF16, [128, 64, 128])
    TC = A.alloc("TC", BF16, [128, 32, 128])
    ZT = A.alloc("ZT", BF16, [128, 8, 128])
    for ri in range(2):
        BB = BBr if ri == 0 else BBi
        bbk = "BBr" if ri == 0 else "BBi"
        for m4 in range(4):
            for dfc in range(8):
                S.op("dve", lambda e, BB=BB, m4=m4, dfc=dfc: e.tensor_tensor(
                    out=ZT[:, dfc, :].rearrange("p (m g c) -> p m g c", m=4, g=2),
                    in0=BB[:, dfc * 4:(dfc + 1) * 4, :].unsqueeze(2).broadcast_to([128, 4, 2, 16]),
                    in1=masksB[:, m4, :].rearrange("p (m g c) -> p m g c", m=4, g=2), op=ALU.mult),
                    r=[bbk, "masksB"], w=[("ZT", dfc)])
            b = 4 + (ri * 4 + m4) % 2
            pvb = psbf(b)
            for dfc in range(8):
                S.op("pe", lambda e, dfc=dfc, pvb=pvb: e.transpose(out=pvb[:, dfc * 128:(dfc + 1) * 128], in_=ZT[:, dfc, :], identity=idb),
                     r=[("ZT", dfc), "idb"], w=[psk(b)])
            base = (ri * 4 + m4) * 8
            copy_op("dve", TB[:, base:base + 8, :], pvb.rearrange("p (a b) -> p a b", a=8), [psk(b)], [("TB", ri, m4)])
    A.free("BBr", "BBi")
    Cn = [A.alloc("Cn%d" % i, F32, [128, 8, 64]) for i in range(2)]
    for ri, cd in enumerate([cre_d, cim_d]):
        for dd in range(2):
            S.op("sp", lambda e, ri=ri, cd=cd, dd=dd: e.dma_start(out=Cn[ri][:, 4 * dd:4 * dd + 4, :], in_=cd[dd].rearrange("(fc g8) co p -> (g8 co) fc p", g8=8)),
                 w=["Cn%d" % ri], dma=True)
    for ri in range(2):
        for par in range(2):
            for dfc in range(8):
                if ri == 0:
                    S.op("dve", lambda e, par=par, dfc=dfc: e.tensor_tensor(
                        out=ZT[:, dfc, :].rearrange("p (g q) -> p g q", g=2),
                        in0=Cn[0][:, dfc, :].unsqueeze(1).broadcast_to([128, 2, 64]),
                        in1=masks[:, 1, par, :].rearrange("p (g q) -> p g q", g=2), op=ALU.mult),
                        r=["Cn0", "masks"], w=[("ZT", dfc)])
                else:
                    S.op("dve", lambda e, par=par, dfc=dfc: e.scalar_tensor_tensor(
                        out=ZT[:, dfc, :].rearrange("p (g q) -> p g q", g=2),
                        in0=Cn[1][:, dfc, :].unsqueeze(1).broadcast_to([128, 2, 64]), scalar=-1.0,
                        in1=masks[:, 1, par, :].rearrange("p (g q) -> p g q", g=2), op0=ALU.mult, op1=ALU.mult),
                        r=["Cn1", "masks"], w=[("ZT", dfc)])
            b = 4 + (ri * 2 + par) % 2
            pvb = psbf(b)
            for dfc in range(8):
                S.op("pe", lambda e, dfc=dfc, pvb=pvb: e.transpose(out=pvb[:, dfc * 128:(dfc + 1) * 128], in_=ZT[:, dfc, :], identity=idb),
                     r=[("ZT", dfc), "idb"], w=[psk(b)])
            base = (ri * 2 + par) * 8
            copy_op("dve", TC[:, base:base + 8, :], pvb.rearrange("p (a b) -> p a b", a=8), [psk(b)], [("TC", ri, par)])
    if dbg == "s5tb":
        return dbg_finish([(TB[:, 0:8, :].rearrange("p a b -> p (a b)"), [("TB", 0, 0)]), (TB[:, 16:24, :].rearrange("p a b -> p (a b)"), [("TB", 1, 0)]),
                           (P5.rearrange("p a b -> p (a b)"), ["P5"])])
    A.free("Cn0", "Cn1", "ZT", "masks", "masksB")

    YS = A.alloc("YS", F32, [128, 4, 2048], top=True)
    for fc in range(4):
        S.op("pool", lambda e, fc=fc: e.tensor_scalar(out=YS[:, fc, :], in0=UT[:, fc, :, :].rearrange("p j b -> p (j b)"), scalar1=gT[:, 40 + fc:41 + fc], scalar2=0.0, op0=ALU.mult, op1=ALU.add),
             r=[("UT", fc, tc) for tc in range(4)] + ["gT"], w=[("YS", fc, tc) for tc in range(4)])

    LB_ = 8
    NBLK = 2048 // LB_
    NQ = 2
    Sst = [A.alloc("Sst%d" % q, BF16, [128, 2, LB_, NBLK]) for q in range(NQ)]
    DG = [A.alloc("DG%d" % q, BF16, [128, 3, 128]) for q in range(2 * NQ)]
    Hb = [A.alloc("Hb%d" % q, BF16, [128, 2, NBLK]) for q in range(NQ)]
    FPs_ = [A.alloc("FPall%d" % g, F32, [128, 8, 2, NBLK]) for g in range(2)]
    bcount = {"n": 0}
    EV = [A.alloc("EV%d" % i, F32, [128, 512]) for i in range(2)]
    TTm = A.alloc("TTm", F32, [128, 4, 128])
    S.op("pool", lambda e: e.memset(TTm[:, 0, 0:8], 0.0), w=[("TTm", 0)])

    def tt(out, in0, in1, op, r, w):
        S.op("dve", lambda e: e.tensor_tensor(out=out, in0=in0, in1=in1, op=op), r=r, w=w)

    def bstep(dst, src, tab, tview, kd, ks, kx):
        T = [tview(q) for q in range(4)]
        tk = [("TTm", q) for q in range(4)]
        kx = list(kx)
        tt(T[0], src[0], tab[0], ALU.mult, [ks[0]] + kx, [tk[0]])
        tt(T[1], src[1], tab[1], ALU.mult, [ks[1]] + kx, [tk[1]])
        tt(T[2], src[1], tab[0], ALU.mult, [ks[1]] + kx, [tk[2]])
        tt(T[3], src[0], tab[1], ALU.mult, [ks[0]] + kx, [tk[3]])
        tt(dst[0], dst[0], T[0], ALU.add, [kd[0], tk[0]], [kd[0]])
        tt(dst[1], dst[1], T[2], ALU.add, [kd[1], tk[2]], [kd[1]])
        tt(dst[0], dst[0], T[1], ALU.subtract, [kd[0], tk[1]], [kd[0]])
        tt(dst[1], dst[1], T[3], ALU.add, [kd[1], tk[3]], [kd[1]])

    def batch_pass(d, items, final, FPall, fpn):
        fwd = (d == 0)
        info = []
        bpar = bcount["n"] % 2
        bcount["n"] += 1
        for q, (i, gp) in enumerate(items):
            c = d * 16 + gp
            fc, m = gp // 4, gp % 4
            h, par = m // 2, m % 2
            dfc = d * 4 + fc
            dq = bpar * NQ + q
            info.append((i, c, fc, m, h, par, dfc, dq))
            for k3, slot in enumerate([ABR, NABI, ABI]):
                S.op("pool", lambda e, dq=dq, k3=k3, slot=slot, c=c: e.tensor_scalar(out=DG[dq][:, k3, :], in0=idb, scalar1=P5[:, slot, c:c + 1], scalar2=0.0, op0=ALU.mult, op1=ALU.add),
                     r=["idb", "P5"], w=[("DG%d" % dq, k3)])
            if final:
                S.op("pool", lambda e, q=q: e.memset(Hb[q], 0.0), w=["Hb%d" % q])
                if fwd:
                    S.op("act", lambda e, q=q, i=i: e.copy(out=Hb[q][:, :, 1:NBLK], in_=FPall[:, i, :, 0:NBLK - 1]), r=[(fpn, i, 0), (fpn, i, 1), "Hb%d" % q], w=["Hb%d" % q])
                else:
                    S.op("act", lambda e, q=q, i=i: e.copy(out=Hb[q][:, :, 0:NBLK - 1], in_=FPall[:, i, :, 1:NBLK]), r=[(fpn, i, 0), (fpn, i, 1), "Hb%d" % q], w=["Hb%d" % q])
        js = list(range(LB_)) if fwd else list(range(LB_ - 1, -1, -1))
        for s_, j in enumerate(js):
            jp = js[s_ - 1] if s_ > 0 else None
            for q, (i, c, fc, m, h, par, dfc, dq) in enumerate(info):
                bank = 4 + bpar * NQ + q
                sk = "Sst%d" % q
                dgk = [("DG%d" % dq, k3) for k3 in range(3)]
                if s_ > 0:
                    prev = (Sst[q][:, 0, jp, :], Sst[q][:, 1, jp, :])
                    pk = [(sk, jp)]
                elif final:
                    prev = (Hb[q][:, 0, :], Hb[q][:, 1, :])
                    pk = ["Hb%d" % q]
                else:
                    prev = None
                    pk = []
                u_rhs = UT[:, fc, j, :]
                uk = [("UT", fc, tc) for tc in range(4)]
                last_bu = prev is None
                for ri in range(2):
                    tbi = (ri * 4 + m) * 8 + dfc
                    S.op("pe", lambda e, ri=ri, tbi=tbi, bank=bank, u_rhs=u_rhs, last_bu=last_bu: e.matmul(
                        PS[:, bank, ri * 256:(ri + 1) * 256], lhsT=TB[:, tbi, :], rhs=u_rhs, start=(ri == 0), stop=last_bu, skip_group_check=True),
                        r=[("TB", ri, m)] + uk, w=[psk(bank)])
                if prev is not None:
                    plan = [(0, 0, 0), (0, 1, 1), (1, 0, 1), (1, 2, 0)]
                    for n_, (ro, k3, rp) in enumerate(plan):
                        S.op("pe", lambda e, dq=dq, ro=ro, k3=k3, rp=rp, bank=bank, prev=prev, n_=n_: e.matmul(
                            PS[:, bank, ro * 256:(ro + 1) * 256], lhsT=DG[dq][:, k3, :], rhs=prev[rp], start=False, stop=(n_ % 2 == 1), skip_group_check=True),
                            r=dgk + pk, w=[psk(bank)])
                S.op("act", lambda e, q=q, j=j, bank=bank: e.copy(out=Sst[q][:, :, j, :], in_=PS[:, bank, :].rearrange("p (r b) -> p r b", r=2)),
                     r=[psk(bank)], w=[(sk, j)])
                if (not final) and s_ == LB_ - 1:
                    S.op("act", lambda e, i=i, bank=bank: e.copy(out=FPall[:, i, :, :], in_=PS[:, bank, :].rearrange("p (r b) -> p r b", r=2)),
                         r=[psk(bank)], w=[(fpn, i, 0), (fpn, i, 1)])
        if not final:
            return
        fc = info[0][2]
        for tc in range(4):
            for q, (i, c, fc_, m, h, par, dfc, dq) in enumerate(info):
                for ri in range(2):
                    tci = (ri * 2 + par) * 8 + dfc
                    S.op("pe", lambda e, q=q, tci=tci, tc=tc, ri=ri, h=h, par=par: e.matmul(
                        PS[64 * h:64 * h + 64, tc, :], lhsT=TC[:, tci, 64 * h:64 * h + 64],
                        rhs=Sst[q][:, ri, 2 * tc:2 * tc + 2, :].rearrange("p j b -> p (j b)"),
                        start=(par == 0 and ri == 0), stop=(par == 1 and ri == 1), skip_group_check=True),
                        r=[("TC", ri, par)] + [("Sst%d" % q, j) for j in range(LB_)], w=[psk(tc)])
            if info[-1][3] == 3:
                ev = EV[tc % 2]
                evk = "EV%d" % (tc % 2)
                S.op("act", lambda e, tc=tc, ev=ev: e.copy(out=ev, in_=PS[:, tc, :]), r=[psk(tc)], w=[evk])
                S.op("pool", lambda e, tc=tc, fc=fc, ev=ev: e.tensor_tensor(out=YS[:, fc, tc * 512:(tc + 1) * 512], in0=ev, in1=YS[:, fc, tc * 512:(tc + 1) * 512], op=ALU.add),
                     r=[evk, ("YS", fc, tc)], w=[("YS", fc, tc)])

    def solve_carries(d, c0, FPall, fpn):
        fwd = (d == 0)
        Fv = [FPall[:, :, ri, :].rearrange("p c (B k) -> p c B k", k=16) for ri in range(2)]
        fk = lambda k: (("FPs", 0, k), ("FPs", 1, k))
        allk = [(fpn, i, ri) for i in range(8) for ri in range(2)]
        S.op("dve", lambda e: e.tensor_copy(out=TTm[:, 0, 0:8], in_=TTm[:, 0, 0:8]), r=allk, w=[("FPs", ri, k) for ri in range(2) for k in range(16)] + [("TTm", 0)])
        tv3 = lambda q: TTm[:, q, :].rearrange("p (c b) -> p c b", c=8)
        a8 = (P5[:, A16R, c0:c0 + 8].unsqueeze(2).broadcast_to([128, 8, 16]), P5[:, A16I, c0:c0 + 8].unsqueeze(2).broadcast_to([128, 8, 16]))
        for k in (range(1, 16) if fwd else range(14, -1, -1)):
            kp = k - 1 if fwd else k + 1
            bstep((Fv[0][:, :, :, k], Fv[1][:, :, :, k]), (Fv[0][:, :, :, kp], Fv[1][:, :, :, kp]), a8, tv3, fk(k), fk(kp), ["P5"])
        kl = 15 if fwd else 0
        a128 = (PW2R[:, c0:c0 + 8, 15], PW2I[:, c0:c0 + 8, 15])
        tv1 = lambda q: TTm[:, q, 0:8]
        for B in (range(1, 16) if fwd else range(14, -1, -1)):
            Bp = B - 1 if fwd else B + 1
            bstep((Fv[0][:, :, B, kl], Fv[1][:, :, B, kl]), (Fv[0][:, :, Bp, kl], Fv[1][:, :, Bp, kl]), a128, tv1, fk(kl), fk(kl), ["PW2R", "PW2I"])
        tv15 = lambda q: TTm[:, q, 0:120].rearrange("p (c b) -> p c b", c=8)
        for k in (range(0, 15) if fwd else range(1, 16)):
            pi_ = k if fwd else 15 - k
            tab = (PW2R[:, c0:c0 + 8, pi_].unsqueeze(2).broadcast_to([128, 8, 15]), PW2I[:, c0:c0 + 8, pi_].unsqueeze(2).broadcast_to([128, 8, 15]))
            if fwd:
                dst = (Fv[0][:, :, 1:16, k], Fv[1][:, :, 1:16, k])
                src = (Fv[0][:, :, 0:15, 15], Fv[1][:, :, 0:15, 15])
            else:
                dst = (Fv[0][:, :, 0:15, k], Fv[1][:, :, 0:15, k])
                src = (Fv[0][:, :, 1:16, 0], Fv[1][:, :, 1:16, 0])
            bstep(dst, src, tab, tv15, fk(k), fk(kl), ["PW2R", "PW2I"])
        S.op("dve", lambda e: e.tensor_copy(out=TTm[:, 0, 0:8], in_=TTm[:, 0, 0:8]), r=[("FPs", ri, k) for ri in range(2) for k in range(16)], w=allk + [("TTm", 0)])

    groups = [(d, fp) for d in range(2) for fp in range(2)]

    def run_pass(g, final):
        d, fp = groups[g]
        gps = [8 * fp + i for i in range(8)]
        for b0 in range(0, 8, NQ):
            batch_pass(d, [(i, gps[i]) for i in range(b0, b0 + NQ)], final, FPs_[g % 2], "FPall%d" % (g % 2))

    def do_solve(g):
        d, fp = groups[g]
        solve_carries(d, d * 16 + 8 * fp, FPs_[g % 2], "FPall%d" % (g % 2))

    run_pass(0, False)
    if dbg == "s5f":
        return dbg_finish([(FPs_[0].rearrange("p a b c -> p (a b c)"), [("FPall0", i, ri) for i in range(8) for ri in range(2)])])
    run_pass(1, False)
    do_solve(0)
    if dbg == "s5c":
        return dbg_finish([(FPs_[0].rearrange("p a b c -> p (a b c)"), [("FPall0", i, ri) for i in range(8) for ri in range(2)])])
    do_solve(1)
    run_pass(0, True)
    run_pass(2, False)
    do_solve(2)
    run_pass(1, True)
    run_pass(3, False)
    do_solve(3)
    run_pass(2, True)
    run_pass(3, True)
    A.free("EV0", "EV1", "Sst0", "Sst1", "DG0", "DG1", "DG2", "DG3", "Hb0", "Hb1", "FPall0", "FPall1", "TTm", "TB", "TC", "PW2R", "PW2I", "P5")
    GLUW = A.alloc("GLUW", BF16, [128, 4, 512])
    gst = A.alloc("gst", F32, [128, 4, 512])
    S.op("sp", lambda e: e.dma_start(out=gst, in_=glu_w_d.rearrange("(k p) f -> p k f", p=128)), w=["gst"], dma=True)
    S.op("pool", lambda e: e.tensor_scalar(out=GLUW, in0=gst, scalar1=1.0, scalar2=0.0, op0=ALU.mult, op1=ALU.add), r=["gst"], w=["GLUW"])
    A.free("gst")

    if dbg == "s5":
        dbg_d = nc.dram_tensor("dbg", [128, 8192], F32, kind="ExternalOutput").ap()
        S.op("sp", lambda e: e.dma_start(out=dbg_d, in_=YS.rearrange("p a b -> p (a b)")), r=[("YS", a, b) for a in range(4) for b in range(4)], w=["o1"], dma=True)
        S.op("sp", lambda e: e.nop(), r=["o1"])
        S.emit(nc)
        es.close()
        return nc

    A.free("UT")
    Z2 = A.alloc("Z2", BF16, [128, 4, 2048])
    gt = [A.alloc("gt%d" % i, F32, [128, 512]) for i in range(4)]
    for tc in range(4):
        for fc in range(4):
            i = (tc * 4 + fc) % 4
            g_ = gt[i]
            gk_ = "gt%d" % i
            xs = YS[:, fc, tc * 512:(tc + 1) * 512]
            yk = ("YS", fc, tc)
            S.op("act", lambda e, g_=g_, xs=xs: e.activation(out=g_, in_=xs, func=AF.Square), r=[yk], w=[gk_])
            S.op("dve", lambda e, g_=g_: e.tensor_scalar(out=g_, in0=g_, scalar1=0.044715, scalar2=1.0, op0=ALU.mult, op1=ALU.add), r=[gk_], w=[gk_])
            S.op("dve", lambda e, g_=g_, xs=xs: e.tensor_tensor(out=g_, in0=g_, in1=xs, op=ALU.mult), r=[gk_, yk], w=[gk_])
            S.op("act", lambda e, g_=g_: e.activation(out=g_, in_=g_, func=AF.Tanh, scale=0.7978845608028654), r=[gk_], w=[gk_])
            S.op("dve", lambda e, g_=g_, xs=xs, fc=fc, tc=tc: e.scalar_tensor_tensor(out=Z2[:, fc, tc * 512:(tc + 1) * 512], in0=g_, scalar=1.0, in1=xs, op0=ALU.add, op1=ALU.mult),
                 r=[gk_, yk], w=[("Z2", fc, tc)])
    if dbg == "gelu":
        return dbg_finish([(Z2[:, 1, :], [("Z2", 1, tc) for tc in range(4)])])
    for tc in range(4):
        for fo in range(4):
            i = (tc * 4 + fo) % 4
            b = 4 + i
            g_ = gt[i]
            gk_ = "gt%d" % i
            for fk in range(4):
                S.op("pe", lambda e, fo=fo, tc=tc, fk=fk, b=b: e.matmul(PS[:, b, :], lhsT=GLUW[:, fk, fo * 128:(fo + 1) * 128], rhs=Z2[:, fk, tc * 512:(tc + 1) * 512],
                                                                        start=(fk == 0), stop=(fk == 3)),
                     r=["GLUW"] + [("Z2", fk, tc)], w=[psk(b)])
            S.op("act", lambda e, g_=g_, fo=fo, b=b: e.activation(out=g_, in_=PS[:, b, :], func=AF.Sigmoid, scale=0.5, bias=gT[:, 44 + fo:45 + fo]),
                 r=[psk(b), "gT"], w=[gk_])
            S.op("dve", lambda e, g_=g_, fo=fo, tc=tc: e.scalar_tensor_tensor(out=YS[:, fo, tc * 512:(tc + 1) * 512], in0=Z2[:, fo, tc * 512:(tc + 1) * 512], scalar=0.5, in1=g_,
                                                                               op0=ALU.mult, op1=ALU.mult),
                 r=[gk_, ("Z2", fo, tc)] + [("YS", f2, tc) for f2 in range(4)], w=[("YS", fo, tc)])
    if dbg == "glu":
        return dbg_finish([(YS[:, 1, :], [("YS", 1, tc) for tc in range(4)])])
    YTs = A.alloc("YTs", BF16, [128, 4, 2048], top=True)
    sqb = A.alloc("sqb", BF16, [128, 4, 512])
    for tc in range(4):
        b = 6 + (tc % 2)
        g_ = gt[tc % 2]
        gk_ = "gt%d" % (tc % 2)
        S.op("act", lambda e, tc=tc: e.activation(out=sqb, in_=YS[:, :, tc * 512:(tc + 1) * 512], func=AF.Square),
             r=[("YS", f2, tc) for f2 in range(4)], w=["sqb"])
        for fk in range(4):
            S.op("pe", lambda e, fk=fk, b=b: e.matmul(PS[:, b, :], lhsT=onesb, rhs=sqb[:, fk, :], start=(fk == 0), stop=(fk == 3)), r=["onesb", "sqb"], w=[psk(b)])
        S.op("act", lambda e, g_=g_, b=b: e.activation(out=g_, in_=PS[:, b, :], func=AF.Sqrt, scale=1.0 / 512, bias=EPS), r=[psk(b)], w=[gk_])
        S.op("dve", lambda e, g_=g_: e.reciprocal(out=g_, in_=g_), r=[gk_], w=[gk_])
        for jj in range(2):
            S.op("dve", lambda e, g_=g_, tc=tc, jj=jj: e.tensor_tensor(out=YTs[:, :, 2 * tc + jj:2048:8], in0=YS[:, :, tc * 512 + jj * 256:tc * 512 + (jj + 1) * 256],
                                                                in1=g_[:, jj * 256:(jj + 1) * 256].unsqueeze(1).broadcast_to([128, 4, 256]), op=ALU.mult),
                 r=[gk_] + [("YS", f2, tc) for f2 in range(4)], w=[("YTs", tc, jj)])
    if dbg == "ssmn":
        return dbg_finish([(YTs[:, 1, :], [("YTs", tc, jj) for tc in range(4) for jj in range(2)])])
    A.free("YS", "Z2", "gt0", "gt1", "gt2", "gt3", "sqb", "GLUW")

    w_open(2, 4)
    wo_ = [load_w(w_out_d[512:1024, dc * 512:(dc + 1) * 512].rearrange("(k p) f -> p k f", p=128), 4, 512, gcol=36) for dc in range(2)]
    for t in range(16):
        for dc in range(2):
            b = (t * 2 + dc) % 4
            for k in range(4):
                S.op("pe", lambda e, t=t, dc=dc, k=k, b=b: e.matmul(PS[:, b, :], lhsT=YTs[:, k, t * 128:(t + 1) * 128], rhs=wo_[dc][0][:, k, :], start=(k == 0), stop=(k == 3)),
                     r=[("YTs", a_, b_) for a_ in range(4) for b_ in range(2)] + [wo_[dc][1]], w=[psk(b)])
            S.op("dve", lambda e, t=t, dc=dc, b=b: e.tensor_tensor(out=X[:, t, dc * 512:(dc + 1) * 512], in0=PS[:, b, :], in1=X[:, t, dc * 512:(dc + 1) * 512], op=ALU.add),
                 r=[psk(b), ("X", t)], w=[("X", t)])
    A.free("YTs")
    if dbg == "wout":
        S.op("sp", lambda e: e.dma_start(out=out_v, in_=X), r=[("X", t) for t in range(16)], w=["o1"], dma=True)
        S.op("sp", lambda e: e.nop(), r=["o1"])
        S.emit(nc)
        es.close()
        return nc

    M_ = A.alloc("M_", F32, [128, 2, 1024])
    S.op("sp", lambda e: e.dma_start(out=M_, in_=mem_d.rearrange("(n p) d -> p n d", p=128)), w=["M_"], dma=True)

    def mtile(t):
        return M_[:, t, :], ["M_"]

    hmT = A.alloc("hmT", BF16, [128, 8, 256])
    rstdm = rms_stats(mtile, 2, 1024, "rstdm")
    norm_transpose(mtile, 2, 8, rstdm, "rstdm", hmT, "hmT", [4, 5], "nm")
    A.free("rstdm", "M_")
    KX = A.alloc("KX", BF16, [128, 8, 256])
    VX = A.alloc("VX", BF16, [128, 2, 1024])
    for half in range(2):
        wb, wk_ = load_w(wk_d[:, half * 512:(half + 1) * 512].rearrange("(k p) f -> p k f", p=128), 8, 512, gcol=16)
        for i in range(4):
            b = 4 + (i % 2)
            for k in range(8):
                S.op("pe", lambda e, wb=wb, i=i, k=k, b=b: e.matmul(PS[:, b, 0:256], lhsT=wb[:, k, i * 128:(i + 1) * 128], rhs=hmT[:, k, :], start=(k == 0), stop=(k == 7)),
                     r=[wk_, ("hmT", 0), ("hmT", 1)], w=[psk(b)])
            copy_op(alt(), KX[:, half * 4 + i, :], PS[:, b, 0:256], [psk(b)], [("KX", half * 4 + i)])
    for half in range(2):
        wb, wk_ = load_w(wv_d[:, half * 512:(half + 1) * 512].rearrange("(k p) f -> p k f", p=128), 8, 512, gcol=16)
        for mt in range(2):
            b = 6 + (mt % 2)
            for k in range(8):
                S.op("pe", lambda e, wb=wb, mt=mt, k=k, b=b: e.matmul(PS[:, b, :], lhsT=hmT[:, k, mt * 128:(mt + 1) * 128], rhs=wb[:, k, :], start=(k == 0), stop=(k == 7)),
                     r=[wk_, ("hmT", mt)], w=[psk(b)])
            copy_op(alt(), VX[:, mt, half * 512:(half + 1) * 512], PS[:, b, :], [psk(b)], [("VX", mt, half)])
    A.free("hmT")
    HT = A.alloc("HTx", BF16, [128, 8, 2048], top=True)
    rstd3 = rms_stats(xtile, 16, 1024, "rstd3")
    norm_transpose(xtile, 16, 8, rstd3, "rstd3", HT, "HTx", [0, 1], "n3")
    A.free("rstd3")
    wq_ = [load_w(wq_d[:, hf * 512:(hf + 1) * 512].rearrange("(k p) f -> p k f", p=128), 8, 512, gcol=8) for hf in range(2)]
    wox = [load_w(wo_d[:, hf * 512:(hf + 1) * 512].rearrange("(k p) f -> p k f", p=128), 8, 512) for hf in range(2)]
    A.free(*wstate["STn"])
    wstate["STn"] = []
    QXs = [A.alloc("QX%d" % i, BF16, [128, 8, 512]) for i in range(2)]
    PX = [A.alloc("PX%d" % i, BF16, [128, 512]) for i in range(4)]
    OT = A.alloc("OT", BF16, [128, 8, 512])
    rsxs = [A.alloc("rsx%d" % i, F32, [128, 512]) for i in range(2)]

    def xa_q(tc):
        QX, qn_ = QXs[tc % 2], "QX%d" % (tc % 2)
        htk = [("HTx", t) for t in range(4 * tc, 4 * tc + 4)]
        for f in range(8):
            b = f % 2
            for k in range(8):
                S.op("pe", lambda e, f=f, k=k, b=b: e.matmul(PS[:, b, :], lhsT=wq_[f // 4][0][:, k, (f % 4) * 128:(f % 4 + 1) * 128], rhs=HT[:, k, tc * 512:(tc + 1) * 512],
                                                             start=(k == 0), stop=(k == 7)),
                     r=htk + [wq_[f // 4][1]], w=[psk(b)])
            copy_op(alt(), QX[:, f, :], PS[:, b, :], [psk(b)], [(qn_, f)])

    def xa_heads(tc):
        for hh in range(4):
            xa_head(tc, hh)

    def xa_head(tc, hh):
        QX, qn_ = QXs[tc % 2], "QX%d" % (tc % 2)
        rsx, rk = rsxs[hh % 2], "rsx%d" % (hh % 2)
        sb_ = 4 if hh % 2 == 0 else 7
        for mt in range(2):
            b = 2 + mt
            pi = (hh % 2) * 2 + mt
            for kk in range(2):
                S.op("pe", lambda e, mt=mt, kk=kk, b=b: e.matmul(PS[:, b, :], lhsT=KX[:, 2 * hh + kk, mt * 128:(mt + 1) * 128], rhs=QX[:, 2 * hh + kk, :],
                                                                 start=(kk == 0), stop=(kk == 1)),
                     r=[("KX", 2 * hh + kk), (qn_, 2 * hh + kk)], w=[psk(b)])
            S.op("act", lambda e, pi=pi, b=b: e.activation(out=PX[pi], in_=PS[:, b, :], func=AF.Exp, scale=1.0 / 16), r=[psk(b)], w=["PX%d" % pi])
        for mt in range(2):
            pi = (hh % 2) * 2 + mt
            S.op("pe", lambda e, mt=mt, pi=pi: e.matmul(PS[:, sb_, :], lhsT=onesb, rhs=PX[pi], start=(mt == 0), stop=(mt == 1)), r=["onesb", "PX%d" % pi], w=[psk(sb_)])
        S.op("dve", lambda e: e.reciprocal(out=rsx, in_=PS[:, sb_, :]), r=[psk(sb_)], w=[rk])
        for ff in range(2):
            b = 5 + ff
            f = 2 * hh + ff
            for mt in range(2):
                pi = (hh % 2) * 2 + mt
                S.op("pe", lambda e, mt=mt, f=f, b=b, pi=pi: e.matmul(PS[:, b, :], lhsT=VX[:, mt, f * 128:(f + 1) * 128], rhs=PX[pi], start=(mt == 0), stop=(mt == 1)),
                     r=[("VX", mt, f // 4), "PX%d" % pi], w=[psk(b)])
            S.op("dve", lambda e, f=f, b=b: e.tensor_tensor(out=OT[:, f, :], in0=PS[:, b, :], in1=rsx, op=ALU.mult), r=[psk(b), rk], w=[("OT", f)])

    def xa_o(tc):
        for tt in range(4):
            t = tc * 4 + tt
            for dc in range(2):
                b = dc
                for k in range(8):
                    S.op("pe", lambda e, tt=tt, dc=dc, k=k, b=b: e.matmul(PS[:, b, :], lhsT=OT[:, k, tt * 128:(tt + 1) * 128], rhs=wox[dc][0][:, k, :], start=(k == 0), stop=(k == 7)),
                         r=[("OT", k), wox[dc][1]], w=[psk(b)])
                S.op("dve", lambda e, t=t, dc=dc, b=b: e.tensor_tensor(out=X[:, t, dc * 512:(dc + 1) * 512], in0=PS[:, b, :], in1=X[:, t, dc * 512:(dc + 1) * 512], op=ALU.add),
                     r=[psk(b), ("X", t)], w=[("X", t)])

    xa_q(0)
    for tc in range(4):
        xa_heads(tc)
        if tc + 1 < 4:
            xa_q(tc + 1)
        xa_o(tc)
    A.free("QX0", "QX1", "PX0", "PX1", "PX2", "PX3", "OT", "rsx0", "rsx1", "KX", "VX", "HTx")

    if dbg == "xattn":
        S.op("sp", lambda e: e.dma_start(out=out_v, in_=X), r=[("X", t) for t in range(16)], w=["o1"], dma=True)
        S.op("sp", lambda e: e.nop(), r=["o1"])
        S.emit(nc)
        es.close()
        return nc

    HT = A.alloc("HTm", BF16, [128, 8, 2048], top=True)
    WR = A.alloc("WR", F32, [128, 8, 20])
    wrs = A.alloc("wrs", F32, [128, 8, 20])
    S.op("sp", lambda e: e.dma_start(out=wrs, in_=wr_d.rearrange("(k p) f -> p k f", p=128)), w=["wrs"], dma=True)
    S.op("pool", lambda e: e.tensor_tensor(out=WR, in0=wrs, in1=gT[:, 24:32].unsqueeze(2).broadcast_to([128, 8, 20]), op=ALU.mult), r=["wrs", "gT"], w=["WR"])
    LOG = A.alloc("LOG", F32, [128, 16, 20])
    rstd4 = rms_stats(xtile, 16, 1024, "rstd4")
    Hfs = [A.alloc("Hf%d" % i, F32, [128, 1024]) for i in range(2)]
    HTFs = [A.alloc("HTF%d" % i, F32, [128, 8, 128]) for i in range(2)]

    def moe_A(t):
        p = t % 2
        Hf, hk = Hfs[p], "Hf%d" % p
        S.op("dve", lambda e: e.tensor_scalar(out=Hf, in0=X[:, t, :], scalar1=rstd4[:, t:t + 1], scalar2=None, op0=ALU.mult), r=[("X", t), ("rstd4", t)], w=[hk])
        for k in range(8):
            b = 2 * p + k // 4
            S.op("pe", lambda e, k=k, b=b: e.transpose(out=PS[:, b, (k % 4) * 128:(k % 4 + 1) * 128], in_=Hf[:, k * 128:(k + 1) * 128], identity=idf),
                 r=[hk, "idf"], w=[psk(b)])

    def moe_B(t):
        p = t % 2
        HTF, fk = HTFs[p], "HTF%d" % p
        for bb in range(2):
            b = 2 * p + bb
            S.op("act", lambda e, b=b, bb=bb: e.copy(out=HT[:, 4 * bb:4 * bb + 4, t * 128:(t + 1) * 128], in_=PS[:, b, :].rearrange("p (k c) -> p k c", k=4)),
                 r=[psk(b)], w=[("HTm", t, bb)])
            S.op("dve", lambda e, b=b, bb=bb: e.tensor_copy(out=HTF[:, 4 * bb:4 * bb + 4, :], in_=PS[:, b, :].rearrange("p (k c) -> p k c", k=4)), r=[psk(b)], w=[(fk, bb)])
        rb_ = 4 + p
        for k in range(8):
            S.op("pe", lambda e, k=k: e.matmul(PS[:, rb_, 0:20], lhsT=HTF[:, k, :], rhs=WR[:, k, :], start=(k == 0), stop=(k == 7)), r=[(fk, k // 4), "WR"], w=[psk(rb_)])
        S.op("dve", lambda e: e.tensor_tensor(out=LOG[:, t, :], in0=PS[:, rb_, 0:20], in1=rbt, op=ALU.add), r=[psk(rb_), "rbt"], w=["LOG"])

    moe_A(0)
    for t in range(16):
        if t + 1 < 16:
            moe_A(t + 1)
        moe_B(t)
    if dbg == "moe_r":
        return dbg_finish([(LOG.rearrange("p a b -> p (a b)"), ["LOG"]), (HT[:, 3, 0:512], [("HTm", t, 0) for t in range(4)])])
    A.free("rstd4", "Hf0", "Hf1", "HTF0", "HTF1", "wrs", "WR")
    GATES = A.alloc("GATES", F32, [128, 16, 16])
    RT = A.alloc("RT", F32, [128, 8, 16, 16])
    gl = LOG[:, :, 0:4]
    el = LOG[:, :, 4:20]
    r4 = lambda i: RT[:, i, :, 0:4]
    r1 = lambda i: RT[:, i, :, 0]
    r16 = lambda i: RT[:, i, :, :]

    def rt(eng, fn):
        S.op(eng, fn, r=["LOG", "RT"], w=["RT"])

    rt("dve", lambda e: e.tensor_reduce(out=r1(0), in_=gl, axis=AX.X, op=ALU.max))
    rt("dve", lambda e: e.tensor_tensor(out=r4(1), in0=gl, in1=r1(0).unsqueeze(2).broadcast_to([128, 16, 4]), op=ALU.subtract))
    rt("act", lambda e: e.activation(out=r4(2), in_=r4(1), func=AF.Exp))
    rt("dve", lambda e: e.tensor_reduce(out=r1(3), in_=r4(2), axis=AX.X, op=ALU.add))
    rt("dve", lambda e: e.reciprocal(out=r1(3), in_=r1(3)))
    rt("dve", lambda e: e.tensor_scalar(out=r4(1), in0=r4(1), scalar1=0.0, scalar2=None, op0=ALU.is_ge))
    rt("dve", lambda e: e.tensor_scalar(out=r4(1), in0=r4(1), scalar1=-1.0, scalar2=1e4, op0=ALU.add, op1=ALU.mult))
    rt("dve", lambda e: e.tensor_tensor(out=r16(4).rearrange("p t (g x) -> p t g x", g=4), in0=el.rearrange("p t (g x) -> p t g x", g=4),
                                        in1=r4(1).unsqueeze(3).broadcast_to([128, 16, 4, 4]), op=ALU.add))
    rt("dve", lambda e: e.tensor_reduce(out=r1(0), in_=r16(4), axis=AX.X, op=ALU.max))
    rt("dve", lambda e: e.tensor_tensor(out=r16(5), in0=r16(4), in1=r1(0).unsqueeze(2).broadcast_to([128, 16, 16]), op=ALU.subtract))
    rt("dve", lambda e: e.tensor_scalar(out=r16(6), in0=r16(5), scalar1=0.0, scalar2=-1e4, op0=ALU.is_ge, op1=ALU.mult))
    rt("dve", lambda e: e.tensor_tensor(out=r16(6), in0=r16(6), in1=r16(5), op=ALU.add))
    rt("dve", lambda e: e.tensor_reduce(out=r1(2), in_=r16(6), axis=AX.X, op=ALU.max))
    rt("dve", lambda e: e.tensor_tensor(out=r16(6), in0=r16(5), in1=r1(2).unsqueeze(2).broadcast_to([128, 16, 16]), op=ALU.is_ge))
    rt("act", lambda e: e.activation(out=r16(7), in_=r16(5), func=AF.Exp))
    rt("act", lambda e: e.activation(out=r1(1), in_=r1(2), func=AF.Exp))
    rt("dve", lambda e: e.tensor_scalar(out=r1(1), in0=r1(1), scalar1=1.0, scalar2=None, op0=ALU.add))
    rt("dve", lambda e: e.reciprocal(out=r1(1), in_=r1(1)))
    rt("dve", lambda e: e.tensor_tensor(out=r1(1), in0=r1(1), in1=r1(3), op=ALU.mult))
    rt("dve", lambda e: e.tensor_tensor(out=r16(7), in0=r16(7), in1=r16(6), op=ALU.mult))
    S.op("dve", lambda e: e.tensor_tensor(out=GATES, in0=r16(7), in1=r1(1).unsqueeze(2).broadcast_to([128, 16, 16]), op=ALU.mult), r=["RT"], w=["GATES"])
    if dbg == "moe_g":
        return dbg_finish([(GATES.rearrange("p a b -> p (a b)"), ["GATES"])])
    A.free("RT", "LOG")

    AT = A.alloc("AT", BF16, [128, 4, 2048])
    sg = [A.alloc("sg%d" % i, F32, [128, 512]) for i in range(2)]
    w_close()
    w_open(2, 4)
    htk_all = lambda tc: [("HTm", t, b) for t in range(4 * tc, 4 * tc + 4) for b in range(2)]
    for ex in range(16):
        wgb, wgk = load_w(wg_d[ex].rearrange("(k p) f -> p k f", p=128), 8, 512, gcol=24)
        wub2, wuk2 = load_w(wu_d[ex].rearrange("(k p) f -> p k f", p=128), 8, 512, gcol=24)
        wdb, wdk = load_w(wd_d[ex].rearrange("(k p) f -> p k f", p=128), 4, 1024)
        for tc in range(4):
            for fc in range(4):
                i = (tc * 4 + fc) % 2
                bg, bu = i, 2 + i
                for k in range(8):
                    S.op("pe", lambda e, wgb=wgb, fc=fc, tc=tc, k=k, bg=bg: e.matmul(PS[:, bg, :], lhsT=wgb[:, k, fc * 128:(fc + 1) * 128], rhs=HT[:, k, tc * 512:(tc + 1) * 512],
                                                                                     start=(k == 0), stop=(k == 7)), r=htk_all(tc) + [wgk], w=[psk(bg)])
                for k in range(8):
                    S.op("pe", lambda e, wub2=wub2, fc=fc, tc=tc, k=k, bu=bu: e.matmul(PS[:, bu, :], lhsT=wub2[:, k, fc * 128:(fc + 1) * 128], rhs=HT[:, k, tc * 512:(tc + 1) * 512],
                                                                                       start=(k == 0), stop=(k == 7)), r=htk_all(tc) + [wuk2], w=[psk(bu)])
                S.op("act", lambda e, i=i, bg=bg: e.activation(out=sg[i], in_=PS[:, bg, :], func=AF.Silu), r=[psk(bg)], w=["sg%d" % i])
                S.op("dve", lambda e, i=i, bu=bu, fc=fc, tc=tc: e.tensor_tensor(out=AT[:, fc, tc * 512:(tc + 1) * 512], in0=PS[:, bu, :], in1=sg[i], op=ALU.mult),
                     r=[psk(bu), "sg%d" % i], w=[("AT", fc, tc)])
        for t in range(16):
            for dc in range(2):
                b = 4 + (t * 2 + dc) % 4
                for fk in range(4):
                    S.op("pe", lambda e, wdb=wdb, t=t, dc=dc, fk=fk, b=b: e.matmul(PS[:, b, :], lhsT=AT[:, fk, t * 128:(t + 1) * 128], rhs=wdb[:, fk, dc * 512:(dc + 1) * 512],
                                                                                   start=(fk == 0), stop=(fk == 3)), r=[("AT", fk, t // 4), wdk], w=[psk(b)])
                S.op("dve", lambda e, t=t, dc=dc, b=b, ex=ex: e.scalar_tensor_tensor(out=X[:, t, dc * 512:(dc + 1) * 512], in0=PS[:, b, :], scalar=GATES[:, t, ex:ex + 1],
                                                                                     in1=X[:, t, dc * 512:(dc + 1) * 512], op0=ALU.mult, op1=ALU.add),
                     r=[psk(b), "GATES", ("X", t)], w=[("X", t)])
    w_close()
    A.free("AT", "sg0", "sg1", "HTm", "GATES")

    gfin = A.alloc("gfin", F32, [128, 1024])
    S.op("sp", lambda e: e.dma_start(out=gfin, in_=gfin_d.partition_broadcast(128)), w=["gfin"], dma=True)
    rstd5 = rms_stats(xtile, 16, 1024, "rstd5")
    outk = []
    for t in range(16):
        S.op("dve", lambda e, t=t: e.scalar_tensor_tensor(out=X[:, t, :], in0=X[:, t, :], scalar=rstd5[:, t:t + 1], in1=gfin, op0=ALU.mult, op1=ALU.mult),
             r=[("X", t), ("rstd5", t), "gfin"], w=[("X", t)])
        if t % 4 == 3:
            i = t // 4
            S.op("sp", lambda e, i=i: e.dma_start(out=out_v[:, 4 * i:4 * i + 4, :], in_=X[:, 4 * i:4 * i + 4, :]), r=[("X", tt) for tt in range(4 * i, 4 * i + 4)], w=["out%d" % i], dma=True)
            outk.append("out%d" % i)
    S.op("sp", lambda e: e.nop(), r=outk)
    S.emit(nc)
    es.close()
    build.peak = A.peak
    return nc


def host_consts():
    ident = np.eye(128, dtype=np.float32)
    t = np.arange(2048)
    row = (t // 64).astype(np.float64)
    col = (t % 64).astype(np.float64)
    freqs = 10000.0 ** (-np.arange(16, dtype=np.float64) / 16)
    ar = row[:, None] * freqs[None, :]
    ac = col[:, None] * freqs[None, :]
    cos = np.concatenate([np.cos(ar), np.cos(ar), np.cos(ac), np.cos(ac)], axis=1).astype(np.float32)
    sin = np.concatenate([-np.sin(ar), np.sin(ar), -np.sin(ac), np.sin(ac)], axis=1).astype(np.float32)
    rope = np.stack([cos.reshape(16, 128, 64).transpose(1, 0, 2), sin.reshape(16, 128, 64).transpose(1, 0, 2)]).astype(np.float32)
    mB = np.zeros((128, 2, 4, 2, 16), np.float32)
    for gl in range(2):
        for par in range(2):
            for m in range(4):
                if m % 2 == par:
                    mB[gl * 64:(gl + 1) * 64, par, m, gl, :] = 1.0
    mC = np.zeros((128, 2, 2, 64), np.float32)
    for g8 in range(8):
        for par in range(2):
            for gl in range(2):
                if (g8 % 4) // 2 == par and g8 % 2 == gl:
                    mC[g8 * 16:(g8 + 1) * 16, par, gl, :] = 1.0
    masks = np.stack([mB.reshape(128, 2, 128), mC.reshape(128, 2, 128)]).astype(np.float32)
    mB4 = np.zeros((128, 4, 4, 2, 16), np.float32)
    for gl in range(2):
        for m in range(4):
            mB4[gl * 64:(gl + 1) * 64, m, m, gl, :] = 1.0
    return ident, rope, masks, mB4.reshape(128, 4, 128)


_CACHE = {}


def make_in_maps(inp):
    f = lambda a: np.ascontiguousarray(np.asarray(a, dtype=np.float32))
    ident, rope, masks, masksB = host_consts()
    vecs = np.concatenate([
        f(inp["mix_norm_g"][0]).reshape(8, 128), f(inp["xa_norm_g"][0]).reshape(8, 128), f(inp["mem_norm_g"][0]).reshape(8, 128),
        f(inp["moe_norm_g"][0]).reshape(8, 128), f(inp["att_out_g"][0]).reshape(4, 128), f(inp["ssm_out_g"][0]).reshape(4, 128),
        f(inp["ssm_d"][0]).reshape(4, 128), f(inp["ssm_glu_b"][0]).reshape(4, 128)], axis=0)
    ls = f(inp["ssm_log_step"][0])
    ls_rep = np.repeat(ls.reshape(2, 16, 2, 1), 64, axis=3).reshape(32, 128)
    s5p = np.concatenate([f(inp["ssm_lambda_re"][0]).reshape(32, 128), f(inp["ssm_lambda_im"][0]).reshape(32, 128), ls_rep], axis=0)
    shared = {
        "w_in": f(inp["w_in"][0]), "w_out": f(inp["w_out"][0]), "glu_w": f(inp["ssm_glu_w"][0]),
        "xa_wq": f(inp["xa_wq"][0]), "xa_wk": f(inp["xa_wk"][0]), "xa_wv": f(inp["xa_wv"][0]), "xa_wo": f(inp["xa_wo"][0]),
        "wr": f(np.concatenate([np.asarray(inp["router_grp_w"][0]), np.asarray(inp["router_exp_w"][0])], axis=1)),
        "wg": f(inp["exp_w_gate"][0]), "wu": f(inp["exp_w_up"][0]), "wd": f(inp["exp_w_down"][0]),
        "vecs": f(vecs), "gfin": f(inp["final_norm_g"]), "gq": f(inp["q_norm_g"][0]), "gk": f(inp["k_norm_g"][0]),
        "rb": f(np.concatenate([np.asarray(inp["router_grp_b"][0]), np.asarray(inp["router_exp_b"][0])])),
        "g64": f(np.asarray(inp["att_out_g"][0]).reshape(8, 64).T),
        "s5p": f(s5p), "b_re": f(inp["ssm_b_re"][0]), "b_im": f(inp["ssm_b_im"][0]), "c_re": f(inp["ssm_c_re"][0]), "c_im": f(inp["ssm_c_im"][0]),
        "ident": ident, "rope": rope, "masks": masks, "masksB": masksB,
    }
    x = f(inp["x"])
    mem = f(inp["mem"])
    maps = []
    for b in range(8):
        m = dict(shared)
        m["x"] = x[b]
        m["mem"] = mem[b]
        maps.append(m)
    return maps


def kernel(**inputs):
    if "nc" not in _CACHE:
        _CACHE["nc"] = build()
    nc = _CACHE["nc"]
    in_maps = make_in_maps(inputs)
    res = run_bass_kernel_spmd(nc, in_maps, core_ids=list(range(8)))
    out = np.stack([np.asarray(res.results[b]["out"], dtype=np.float32) for b in range(8)], axis=0)
    return out
```

```python
import numpy as np
from contextlib import ExitStack
import concourse.bass as bass
import concourse.mybir as mybir
from concourse.alu_op_type import AluOpType as ALU
from concourse.bass_utils import run_bass_kernel_spmd

AF = mybir.ActivationFunctionType
F32 = mybir.dt.float32
BF16 = mybir.dt.bfloat16
U8 = mybir.dt.uint8
AX = mybir.AxisListType
EPS = 1e-6


class Rec:
    __slots__ = ("eng", "fn", "deps", "signal", "semval", "is_dma", "dsem", "dval", "prev_dma")

    def __init__(self, eng, fn, deps, is_dma):
        self.eng = eng
        self.fn = fn
        self.deps = deps
        self.signal = False
        self.semval = 0
        self.is_dma = is_dma
        self.dsem = None
        self.dval = 0
        self.prev_dma = None


def _buf(k):
    return k if isinstance(k, str) else k[0]


class Sched:
    ENG = ["pe", "act", "dve", "pool", "sp"]
    NEAR = 10 ** 9

    def __init__(self):
        self.q = {e: [] for e in self.ENG}
        self.lastw = {}
        self.readers = {}
        self.alias = {}
        self.touch = {}

    def op(self, eng, fn, r=(), w=(), dma=False):
        w = list(w) + [k for k in r if _buf(k) == "PS"]
        r = [k for k in r if _buf(k) != "PS"]
        idx = len(self.q[eng])
        deps = set()
        for k in r:
            lw = self.lastw.get(k)
            if lw is not None:
                deps.add(lw)
        for k in w:
            lw = self.lastw.get(k)
            if lw is not None:
                deps.add(lw)
            for rd in self.readers.get(k, ()):
                deps.add(rd)
        bufs = set(_buf(k) for k in r) | set(_buf(k) for k in w)
        for b in bufs:
            for a in self.alias.get(b, ()):
                t = self.touch.get(a)
                if t:
                    for e2, i2 in t["c"].items():
                        deps.add((e2, i2))
                    for d in t["d"]:
                        deps.add(d)
        rec = Rec(eng, fn, deps, dma)
        self.q[eng].append(rec)
        me = (eng, idx)
        for k in w:
            self.lastw[k] = me
            self.readers[k] = []
        for k in r:
            self.readers.setdefault(k, []).append(me)
        for b in bufs:
            t = self.touch.setdefault(b, {"c": {}, "d": set()})
            if dma:
                t["d"].add(me)
            else:
                t["c"][eng] = idx
        return rec

    def emit(self, nc, ndsem=24):
        q = self.q
        for e in self.ENG:
            for idx, rec in enumerate(q[e]):
                nd = set()
                best = {}
                for (pe_, pi) in rec.deps:
                    prod = q[pe_][pi]
                    if prod.is_dma:
                        nd.add((pe_, pi))
                        continue
                    if pe_ == e:
                        if e == "pe":
                            continue
                        if idx - pi > self.NEAR:
                            continue
                    if pe_ not in best or best[pe_] < pi:
                        best[pe_] = pi
                for pe_, pi in best.items():
                    nd.add((pe_, pi))
                rec.deps = nd
                for (pe_, pi) in nd:
                    q[pe_][pi].signal = True
        with ExitStack() as es:
            sems = {e: es.enter_context(nc.semaphore("s_" + e)) for e in self.ENG}
            dsems = [es.enter_context(nc.semaphore("d%d" % i)) for i in range(ndsem)]
            for e in self.ENG:
                c = 0
                for rec in q[e]:
                    if rec.is_dma:
                        continue
                    if rec.signal:
                        c += 1
                        rec.semval = c
            dcount = [0] * ndsem
            dlast = [None] * ndsem
            di = 0
            for e in self.ENG:
                for rec in q[e]:
                    if rec.is_dma:
                        s = di % ndsem
                        di += 1
                        dcount[s] += 1
                        rec.dsem = s
                        rec.dval = 16 * dcount[s]
                        rec.prev_dma = dlast[s]
                        dlast[s] = rec
            block = es.enter_context(nc.Block())

            def make(e):
                def body(engine):
                    waited = {}
                    for rec in q[e]:
                        waits = []
                        for (pe_, pi) in rec.deps:
                            prod = q[pe_][pi]
                            if prod.is_dma:
                                waits.append(("d", prod.dsem, prod.dval))
                            else:
                                waits.append(("c", pe_, prod.semval))
                        if rec.is_dma and rec.prev_dma is not None:
                            waits.append(("d", rec.prev_dma.dsem, rec.prev_dma.dval))
                        for (kind, s, v) in waits:
                            key = (kind, s)
                            if waited.get(key, 0) >= v:
                                continue
                            waited[key] = v
                            engine.wait_ge(dsems[s] if kind == "d" else sems[s], v)
                        ins = rec.fn(engine)
                        if rec.is_dma:
                            ins.then_inc(dsems[rec.dsem], 16)
                        elif rec.signal:
                            ins.then_inc(sems[e], 1)
                return body

            block.tensor(make("pe"))
            block.scalar(make("act"))
            block.vector(make("dve"))
            block.gpsimd(make("pool"))
            block.sync(make("sp"))


class Arena:
    def __init__(self, S, g, size):
        self.S = S
        self.g = g
        self.size = size
        self.live = {}
        self.dead = []
        self.peak = 0

    def alloc(self, name, dt, shape, top=False):
        esz = 4 if dt == F32 else 2
        n = int(np.prod(shape[1:])) * esz
        nb = (n + 63) // 64 * 64
        ivs = sorted(self.live.values())
        gaps = []
        cur = 0
        for (o, s) in ivs:
            if o > cur:
                gaps.append((cur, o))
            cur = max(cur, o + s)
        if cur < self.size:
            gaps.append((cur, self.size))
        fit = [(a, b) for (a, b) in gaps if b - a >= nb]
        assert fit, ("arena overflow", name, nb, gaps, sorted((v, k) for k, v in self.live.items()))
        if top:
            a, b = fit[-1]
            off = b - nb
        else:
            off = fit[0][0]
        assert off + nb <= self.size, ("arena overflow", name, off, nb, sorted((v, k) for k, v in self.live.items()))
        assert name not in self.live and name not in self.S.touch, name
        self.live[name] = (off, nb)
        self.peak = max(self.peak, off + nb)
        self.S.alias[name] = [nm for (o, s, nm) in self.dead if o < off + nb and off < o + s]
        self.dead = [(o, s, nm) for (o, s, nm) in self.dead if not (off <= o and o + s <= off + nb)]
        v = self.g[0:shape[0], off:off + n].bitcast(dt)
        if len(shape) == 3:
            v = v.rearrange("p (a b) -> p a b", a=shape[1])
        elif len(shape) == 4:
            v = v.rearrange("p (a b c) -> p a b c", a=shape[1], b=shape[2])
        return v

    def free(self, *names):
        for name in names:
            off, s = self.live.pop(name)
            self.dead.append((off, s, name))


def build(dbg=None):
    nc = bass.Bass("TRN2", target_bir_lowering=False)

    def din(name, shape):
        return nc.dram_tensor(name, list(shape), F32, kind="ExternalInput").ap()

    x_d = din("x", [2048, 1024])
    mem_d = din("mem", [256, 1024])
    w_in_d = din("w_in", [1024, 1280])
    w_out_d = din("w_out", [1024, 1024])
    glu_w_d = din("glu_w", [512, 512])
    wq_d = din("xa_wq", [1024, 1024])
    wk_d = din("xa_wk", [1024, 1024])
    wv_d = din("xa_wv", [1024, 1024])
    wo_d = din("xa_wo", [1024, 1024])
    wr_d = din("wr", [1024, 20])
    wg_d = din("wg", [16, 1024, 512])
    wu_d = din("wu", [16, 1024, 512])
    wd_d = din("wd", [16, 512, 1024])
    vecs_d = din("vecs", [48, 128])
    gfin_d = din("gfin", [1024])
    gq_d = din("gq", [64])
    gk_d = din("gk", [64])
    rb_d = din("rb", [20])
    g64_d = din("g64", [64, 8])
    s5p_d = din("s5p", [96, 128])
    bre_d = din("b_re", [2, 32, 64, 16])
    bim_d = din("b_im", [2, 32, 64, 16])
    cre_d = din("c_re", [2, 32, 16, 64])
    cim_d = din("c_im", [2, 32, 16, 64])
    ident_d = din("ident", [128, 128])
    rope_d = din("rope", [2, 128, 16, 64])
    masks_d = din("masks", [2, 128, 2, 128])
    masksB_d = din("masksB", [128, 4, 128])
    out_d = nc.dram_tensor("out", [2048, 1024], F32, kind="ExternalOutput").ap()
    dbg_d = None

    S = Sched()
    es = ExitStack()
    GSZ = 207 * 1024
    G = es.enter_context(nc.sbuf_tensor("G", [128, GSZ], U8))
    PS = es.enter_context(nc.psum_tensor("PS", [128, 8, 512], F32))
    A = Arena(S, G, GSZ)

    def psk(b):
        return ("PS", b)

    def psbf(b):
        return PS[:, b, :].bitcast(BF16)


    def dbg_finish(items):
        tot = sum(int(ap.shape[1]) for ap, _ in items)
        dd = nc.dram_tensor("dbg", [128, tot], F32, kind="ExternalOutput").ap()
        off = 0
        oks = []
        for i, (ap, keys) in enumerate(items):
            n = int(ap.shape[1])
            if ap.dtype == F32:
                S.op("sp", lambda e, ap=ap, off=off, n=n: e.dma_start(out=dd[:, off:off + n], in_=ap), r=keys, w=["dbgo%d" % i], dma=True)
            else:
                tmp = A.alloc("dbgtmp%d" % i, F32, [128, n])
                S.op("dve", lambda e, tmp=tmp, ap=ap: e.tensor_copy(out=tmp, in_=ap), r=keys, w=["dbgtmp%d" % i])
                S.op("sp", lambda e, tmp=tmp, off=off, n=n: e.dma_start(out=dd[:, off:off + n], in_=tmp), r=["dbgtmp%d" % i], w=["dbgo%d" % i], dma=True)
            oks.append("dbgo%d" % i)
            off += n
        S.op("sp", lambda e: e.nop(), r=oks)
        S.emit(nc)
        es.close()
        return nc

    idf = A.alloc("idf", F32, [128, 128])
    idb = A.alloc("idb", BF16, [128, 128])
    onesb = A.alloc("onesb", BF16, [128, 128])
    gT = A.alloc("gT", F32, [128, 48])
    rbt = A.alloc("rbt", F32, [128, 20])
    gq8 = A.alloc("gq8", F32, [128, 64])
    gkt = A.alloc("gkt", F32, [128, 64])
    X = A.alloc("X", F32, [128, 16, 1024], top=True)
    x_v = x_d.rearrange("(n p) d -> p n d", p=128)
    out_v = out_d.rearrange("(n p) d -> p n d", p=128)

    S.op("sp", lambda e: e.dma_start(out=idf, in_=ident_d), w=["idf"], dma=True)
    for i in range(4):
        S.op("sp", lambda e, i=i: e.dma_start(out=X[:, 4 * i:4 * i + 4, :], in_=x_v[:, 4 * i:4 * i + 4, :]),
             w=[("X", t) for t in range(4 * i, 4 * i + 4)], dma=True)
    vecs_sb = A.alloc("vecs_sb", F32, [128, 128])
    S.op("sp", lambda e: e.dma_start(out=vecs_sb[0:48, :], in_=vecs_d), w=["vecs_sb"], dma=True)
    S.op("sp", lambda e: e.dma_start(out=rbt, in_=rb_d.partition_broadcast(128)), w=["rbt"], dma=True)
    S.op("sp", lambda e: e.dma_start(out=gq8, in_=gq_d.partition_broadcast(128)), w=["gq8"], dma=True)
    S.op("sp", lambda e: e.dma_start(out=gkt, in_=gk_d.partition_broadcast(128)), w=["gkt"], dma=True)
    S.op("dve", lambda e: e.tensor_copy(out=idb, in_=idf), r=["idf"], w=["idb"])
    S.op("pool", lambda e: e.memset(onesb, 1.0), w=["onesb"])
    S.op("pe", lambda e: e.transpose(out=PS[:, 7, 0:48], in_=vecs_sb[0:48, :], identity=idf[0:48, 0:48]),
         r=["vecs_sb", "idf"], w=[psk(7)])
    S.op("dve", lambda e: e.tensor_copy(out=gT, in_=PS[:, 7, 0:48]), r=[psk(7)], w=["gT"])
    S.op("dve", lambda e: e.tensor_scalar(out=gq8, in0=gq8, scalar1=0.125, scalar2=None, op0=ALU.mult), r=["gq8"], w=["gq8"])
    A.free("vecs_sb")

    if dbg == "s0":
        return dbg_finish([(gT, ["gT"]), (gq8, ["gq8"]), (gkt, ["gkt"]), (rbt, ["rbt"]), (X[:, 3, :], [("X", 3)])])
    wstate = {"st": 0, "wb": 0, "nst": 0, "nwb": 0, "ST": None, "WB": None}

    def w_open(nst, nwb):
        wstate["ST"] = [A.alloc("ST%d_%d" % (i, wstate["nst"] + 1000 * nst), F32, [128, 4096]) for i in range(nst)]
        wstate["STn"] = ["ST%d_%d" % (i, wstate["nst"] + 1000 * nst) for i in range(nst)]
        wstate["WB"] = [A.alloc("WB%d_%d" % (i, wstate["nwb"] + 1000 * nwb), BF16, [128, 4096]) for i in range(nwb)]
        wstate["WBn"] = ["WB%d_%d" % (i, wstate["nwb"] + 1000 * nwb) for i in range(nwb)]
        wstate["nst"] += 1
        wstate["nwb"] += 1
        wstate["st"] = 0
        wstate["wb"] = 0

    def w_close():
        A.free(*wstate["STn"])
        A.free(*wstate["WBn"])

    def load_w(src, a, b, gcol=None, eng="pool"):
        si = wstate["st"] % len(wstate["ST"])
        wi = wstate["wb"] % len(wstate["WB"])
        wstate["st"] += 1
        wstate["wb"] += 1
        st = wstate["ST"][si][:, 0:a * b].rearrange("p (a b) -> p a b", a=a)
        wb = wstate["WB"][wi][:, 0:a * b].rearrange("p (a b) -> p a b", a=a)
        sk = wstate["STn"][si]
        wk = wstate["WBn"][wi]
        S.op("sp", lambda e: e.dma_start(out=st, in_=src), w=[sk], dma=True)
        if gcol is None:
            S.op(eng, lambda e: e.tensor_scalar(out=wb, in0=st, scalar1=1.0, scalar2=0.0, op0=ALU.mult, op1=ALU.add), r=[sk], w=[wk])
        else:
            S.op(eng, lambda e: e.tensor_tensor(out=wb, in0=st, in1=gT[:, gcol:gcol + a].unsqueeze(2).broadcast_to([128, a, b]), op=ALU.mult),
                 r=[sk, "gT"], w=[wk])
        return wb, wk

    cnt = {"n": 0}

    def alt():
        cnt["n"] += 1
        return "act" if cnt["n"] % 2 else "dve"

    def copy_op(eng, out, in_, r, w):
        if eng == "act":
            S.op("act", lambda e: e.copy(out=out, in_=in_), r=r, w=w)
        else:
            S.op(eng, lambda e: e.tensor_copy(out=out, in_=in_), r=r, w=w)

    def rms_stats(src_tiles, ntile, width, name, group=4):
        ss = A.alloc(name + "_ss", F32, [128, ntile])
        rstd = A.alloc(name, F32, [128, ntile])
        junk = A.alloc(name + "_junk", BF16, [128, width])
        for t0 in range(0, ntile, group):
            for t in range(t0, min(ntile, t0 + group)):
                ap, keys = src_tiles(t)
                S.op("act", lambda e, ap=ap, t=t: e.activation(out=junk, in_=ap, func=AF.Square, accum_out=ss[:, t:t + 1]),
                     r=keys, w=[name + "_junk", (name + "_ss", t)])
            t1 = min(ntile, t0 + group)
            S.op("act", lambda e, t0=t0, t1=t1: e.activation(out=rstd[:, t0:t1], in_=ss[:, t0:t1], func=AF.Sqrt, scale=1.0 / width, bias=EPS),
                 r=[(name + "_ss", t) for t in range(t0, t1)], w=[(name, t) for t in range(t0, t1)])
            S.op("dve", lambda e, t0=t0, t1=t1: e.reciprocal(out=rstd[:, t0:t1], in_=rstd[:, t0:t1]),
                 r=[(name, t) for t in range(t0, t1)], w=[(name, t) for t in range(t0, t1)])
        A.free(name + "_ss", name + "_junk")
        return rstd

    def norm_transpose(src_tiles, ntile, nk, rstd, rname, dst, dname, banks, tmpname):
        hn = [A.alloc("%s_hn%d" % (tmpname, i), BF16, [128, nk * 128]) for i in range(2)]
        for t in range(ntile):
            ap, keys = src_tiles(t)
            h = hn[t % 2]
            hk = "%s_hn%d" % (tmpname, t % 2)
            eng = alt()
            if eng == "act":
                S.op("act", lambda e, ap=ap, h=h, t=t: e.activation(out=h, in_=ap, func=AF.Identity, scale=rstd[:, t:t + 1]),
                     r=keys + [(rname, t)], w=[hk])
            else:
                S.op("dve", lambda e, ap=ap, h=h, t=t: e.tensor_scalar(out=h, in0=ap, scalar1=rstd[:, t:t + 1], scalar2=None, op0=ALU.mult),
                     r=keys + [(rname, t)], w=[hk])
            b = banks[t % len(banks)]
            pv = psbf(b)
            for k in range(nk):
                S.op("pe", lambda e, k=k, h=h, pv=pv: e.transpose(out=pv[:, k * 128:(k + 1) * 128], in_=h[:, k * 128:(k + 1) * 128], identity=idb),
                     r=[hk, "idb"], w=[psk(b)])
            copy_op("dve", dst[:, 0:nk, t * 128:(t + 1) * 128], pv[:, 0:nk * 128].rearrange("p (k c) -> p k c", k=nk), [psk(b)], [(dname, t)])
        A.free("%s_hn0" % tmpname, "%s_hn1" % tmpname)

    def xtile(t):
        return X[:, t, :], [("X", t)]

    HT = A.alloc("HT", BF16, [128, 8, 2048], top=True)
    w_open(2, 3)
    wqb, wqk = load_w(w_in_d[:, 0:512].rearrange("(k p) f -> p k f", p=128), 8, 512, gcol=0)
    wkvb, wkvk = load_w(w_in_d[:, 512:768].rearrange("(k p) f -> p k f", p=128), 8, 256, gcol=0)
    rstd1 = rms_stats(xtile, 16, 1024, "rstd1")
    norm_transpose(xtile, 16, 8, rstd1, "rstd1", HT, "HT", [0, 1], "n1")
    A.free("rstd1")
    if dbg == "s1":
        return dbg_finish([(HT[:, 2, 0:1024], [("HT", t) for t in range(8)]), (wqb[:, 3, :], [wqk])])
    wub, wuk = load_w(w_in_d[:, 768:1280].rearrange("(k p) f -> p k f", p=128), 8, 512, gcol=0)

    ropeC = A.alloc("ropeC", F32, [128, 16, 64])
    ropeS = A.alloc("ropeS", F32, [128, 16, 64])
    S.op("sp", lambda e: e.dma_start(out=ropeC, in_=rope_d[0]), w=["ropeC"], dma=True)
    S.op("sp", lambda e: e.dma_start(out=ropeS, in_=rope_d[1]), w=["ropeS"], dma=True)
    QT = A.alloc("QT", BF16, [128, 4, 2048])
    KT = A.alloc("KT", BF16, [128, 2, 2048])
    S.op("pool", lambda e: e.memset(KT, 0.0), w=["KT"])
    VA = A.alloc("VA", BF16, [128, 16, 2, 128])
    S.op("pool", lambda e: e.memset(VA, 0.0), w=["VA"])
    S.op("pool", lambda e: e.memset(VA[:, :, :, 64:65], 1.0), r=["VA"], w=["VA"])
    A.free(*wstate["STn"])
    wstate["STn"] = []
    sq = [A.alloc("qk_sq%d" % i, F32, [128, 10, 64]) for i in range(2)]
    n0s = [A.alloc("qk_n0%d" % i, F32, [128, 10, 64]) for i in range(2)]
    n1s = [A.alloc("qk_n1%d" % i, F32, [128, 10, 64]) for i in range(2)]
    qbs = [A.alloc("qk_b%d" % i, F32, [128, 10, 64]) for i in range(2)]
    qsss = [A.alloc("qk_ss%d" % i, F32, [128, 10]) for i in range(2)]
    qtm = [A.alloc("qk_tm%d" % i, BF16, [128, 5, 2, 64]) for i in range(2)]

    def st2_A(t):
        p = t % 2
        bq, bkv = 2 + p, 4 + p
        for k in range(8):
            S.op("pe", lambda e, k=k: e.matmul(PS[:, bq, :], lhsT=HT[:, k, t * 128:(t + 1) * 128], rhs=wqb[:, k, :], start=(k == 0), stop=(k == 7)),
                 r=[("HT", t), wqk], w=[psk(bq)])
        for k in range(8):
            S.op("pe", lambda e, k=k: e.matmul(PS[:, bkv, 0:256], lhsT=HT[:, k, t * 128:(t + 1) * 128], rhs=wkvb[:, k, :], start=(k == 0), stop=(k == 7)),
                 r=[("HT", t), wkvk], w=[psk(bkv)])
        qv = PS[:, bq, :].rearrange("p (h d) -> p h d", h=8)
        kv_ = PS[:, bkv, 0:128].rearrange("p (h d) -> p h d", h=2)
        sqk, ssk, n0k = "qk_sq%d" % p, "qk_ss%d" % p, "qk_n0%d" % p
        S.op("act", lambda e: e.activation(out=sq[p][:, 0:8, :], in_=qv, func=AF.Square), r=[psk(bq)], w=[sqk])
        S.op("act", lambda e: e.activation(out=sq[p][:, 8:10, :], in_=kv_, func=AF.Square), r=[psk(bkv)], w=[sqk])
        S.op("dve", lambda e: e.tensor_reduce(out=qsss[p], in_=sq[p], axis=AX.X, op=ALU.add), r=[sqk], w=[ssk])
        S.op("act", lambda e: e.activation(out=qsss[p], in_=qsss[p], func=AF.Sqrt, scale=1.0 / 64, bias=EPS), r=[ssk], w=[ssk])
        S.op("dve", lambda e: e.reciprocal(out=qsss[p], in_=qsss[p]), r=[ssk], w=[ssk])
        S.op("dve", lambda e: e.tensor_tensor(out=n0s[p][:, 0:8, :], in0=qv, in1=qsss[p][:, 0:8].unsqueeze(2).broadcast_to([128, 8, 64]), op=ALU.mult),
             r=[psk(bq), ssk], w=[n0k])
        S.op("dve", lambda e: e.tensor_tensor(out=n0s[p][:, 8:10, :], in0=kv_, in1=qsss[p][:, 8:10].unsqueeze(2).broadcast_to([128, 2, 64]), op=ALU.mult),
             r=[psk(bkv), ssk], w=[n0k])
        S.op("act", lambda e: e.copy(out=VA[:, t, :, 0:64], in_=PS[:, bkv, 128:256].rearrange("p (h d) -> p h d", h=2)),
             r=[psk(bkv), "VA"], w=[("VA", t)])

    def st2_B(t):
        p = t % 2
        n0, n1, qb_ = n0s[p], n1s[p], qbs[p]
        n0k, n1k, qbk, tmk = "qk_n0%d" % p, "qk_n1%d" % p, "qk_b%d" % p, "qk_tm%d" % p
        tm = qtm[p]
        S.op("pool", lambda e: e.tensor_tensor(out=n1[:, 0:8, :], in0=n0[:, 0:8, :], in1=gq8.unsqueeze(1).broadcast_to([128, 8, 64]), op=ALU.mult),
             r=[n0k, "gq8"], w=[n1k])
        S.op("pool", lambda e: e.tensor_tensor(out=n1[:, 8:10, :], in0=n0[:, 8:10, :], in1=gkt.unsqueeze(1).broadcast_to([128, 2, 64]), op=ALU.mult),
             r=[n0k, "gkt"], w=[n1k])
        S.op("dve", lambda e: e.tensor_tensor(out=n0, in0=n1, in1=ropeC[:, t, :].unsqueeze(1).broadcast_to([128, 10, 64]), op=ALU.mult),
             r=[n1k, "ropeC"], w=[n0k])
        n1v = n1.rearrange("p h (b f d) -> p h b f d", b=2, f=2)
        qbv = qb_.rearrange("p h (b f d) -> p h b f d", b=2, f=2)
        for f in range(2):
            S.op("pool", lambda e, f=f: e.tensor_tensor(
                out=qbv[:, :, :, f, :], in0=n1v[:, :, :, 1 - f, :],
                in1=ropeS[:, t, :].rearrange("p (b f d) -> p b f d", b=2, f=2)[:, :, f, :].unsqueeze(1).broadcast_to([128, 10, 2, 16]),
                op=ALU.mult), r=[n1k, "ropeS"], w=[(qbk, f)])
        S.op("dve", lambda e: e.tensor_tensor(out=tm[:, 0:4, :, :], in0=n0[:, 0:8, :].rearrange("p (kv j) d -> p j kv d", kv=2),
                                              in1=qb_[:, 0:8, :].rearrange("p (kv j) d -> p j kv d", kv=2), op=ALU.add),
             r=[n0k, (qbk, 0), (qbk, 1)], w=[(tmk, 0)])
        S.op("dve", lambda e: e.tensor_tensor(out=tm[:, 4, :, :], in0=n0[:, 8:10, :], in1=qb_[:, 8:10, :], op=ALU.add),
             r=[n0k, (qbk, 0), (qbk, 1)], w=[(tmk, 1)])

    def st2_C(t):
        p = t % 2
        tm, tmk = qtm[p], "qk_tm%d" % p
        bt = 6 + p
        pv = psbf(bt)
        for j in range(5):
            S.op("pe", lambda e, j=j: e.transpose(out=pv[:, j * 128:(j + 1) * 128], in_=tm[:, j, :, :].rearrange("p a d -> p (a d)"), identity=idb),
                 r=[(tmk, 0), (tmk, 1), "idb"], w=[psk(bt)])
        copy_op("dve", QT[:, :, t * 128:(t + 1) * 128], pv[:, 0:512].rearrange("p (k c) -> p k c", k=4), [psk(bt)], [("QT", t)])
        copy_op("dve", KT[0:64, 0, t * 128:(t + 1) * 128], pv[0:64, 512:640], [psk(bt), "KT"], [("KT", t, 0)])
        copy_op("dve", KT[64:128, 1, t * 128:(t + 1) * 128], pv[64:128, 512:640], [psk(bt), "KT"], [("KT", t, 1)])

    st2_A(0)
    st2_A(1)
    st2_B(0)
    for t in range(16):
        if t + 2 < 16:
            st2_A(t + 2)
        if t + 1 < 16:
            st2_B(t + 1)
        st2_C(t)
    A.free("ropeC", "ropeS", "qk_sq0", "qk_sq1", "qk_n00", "qk_n01", "qk_n10", "qk_n11", "qk_b0", "qk_b1", "qk_ss0", "qk_ss1", "qk_tm0", "qk_tm1")

    UT = A.alloc("UT", BF16, [128, 4, 8, 256], top=True)
    for fc in range(4):
        for tc in range(4):
            b = (fc * 4 + tc) % 2
            for k in range(8):
                S.op("pe", lambda e, fc=fc, tc=tc, k=k, b=b: e.matmul(PS[:, b, :], lhsT=wub[:, k, fc * 128:(fc + 1) * 128], rhs=HT[:, k, tc * 512:(tc + 1) * 512],
                                                                      start=(k == 0), stop=(k == 7)),
                     r=[("HT", t) for t in range(4 * tc, 4 * tc + 4)] + [wuk], w=[psk(b)])
            copy_op(alt(), UT[:, fc, :, tc * 64:(tc + 1) * 64], PS[:, b, :].rearrange("p (b j) -> p j b", j=8), [psk(b)], [("UT", fc, tc)])
    w_close()
    A.free("HT")

    if dbg == "stage2":
        dbg_d = nc.dram_tensor("dbg", [128, 4 * 2048 + 2048 + 16 * 130 + 4 * 2048], F32, kind="ExternalOutput").ap()
        dt_ = A.alloc("dbgt", F32, [128, 4 * 2048])
        S.op("dve", lambda e: e.tensor_copy(out=dt_, in_=QT.rearrange("p a b -> p (a b)")), r=[("QT", t) for t in range(16)], w=["dbgt"])
        S.op("sp", lambda e: e.dma_start(out=dbg_d[:, 0:8192], in_=dt_), r=["dbgt"], w=["o1"], dma=True)
        S.op("dve", lambda e: e.tensor_tensor(out=dt_[:, 0:2048], in0=KT[:, 0, :], in1=KT[:, 1, :], op=ALU.add), r=[("KT", t, kk) for t in range(16) for kk in range(2)] + ["o1"], w=["dbgt"])
        S.op("sp", lambda e: e.dma_start(out=dbg_d[:, 8192:8192 + 2048], in_=dt_[:, 0:2048]), r=["dbgt"], w=["o2"], dma=True)
        S.op("dve", lambda e: e.tensor_copy(out=dt_[:, 0:2080].rearrange("p (a c) -> p a c", c=65), in_=VA.rearrange("p a b c -> p (a b) c")[:, :, 0:65]), r=[("VA", t) for t in range(16)] + ["o2"], w=["dbgt"])
        S.op("sp", lambda e: e.dma_start(out=dbg_d[:, 10240:10240 + 2080], in_=dt_[:, 0:2080]), r=["dbgt"], w=["o3"], dma=True)
        S.op("dve", lambda e: e.tensor_copy(out=dt_, in_=UT.rearrange("p a j b -> p (a j b)")), r=[("UT", a, b) for a in range(4) for b in range(4)] + ["o3"], w=["dbgt"])
        S.op("sp", lambda e: e.dma_start(out=dbg_d[:, 12320:12320 + 8192], in_=dt_), r=["dbgt"], w=["o4"], dma=True)
        S.op("sp", lambda e: e.nop(), r=["o1", "o2", "o3", "o4"])
        S.emit(nc)
        es.close()
        return nc

    YA = A.alloc("YA", BF16, [128, 8, 2048], top=True)
    S.op("pool", lambda e: e.memset(YA[64:128, :, :], 0.0), w=["YA"])
    PT = [A.alloc("PT%d" % i, BF16, [128, 512]) for i in range(5)]
    SBANK = [0, 1, 7, 5]
    SR = [A.alloc("SR%d" % i, F32, [128, 512]) for i in range(2)]
    RI = [A.alloc("RI%d" % i, F32, [128, 512]) for i in range(2)]
    onesf = A.alloc("onesf", F32, [128, 64])
    S.op("pool", lambda e: e.memset(onesf, 0.0), w=["onesf"])
    S.op("pool", lambda e: e.memset(onesf[64:65, :], 1.0), r=["onesf"], w=["onesf"])
    for i_ in range(2):
        S.op("pool", lambda e, i_=i_: e.memset(SR[i_], 0.0), w=["SR%d" % i_])
    its = [(qc, j, kv, kt) for qc in range(4) for j in range(4) for kv in range(2) for kt in range(16)]
    deferred = []
    g64 = A.alloc("g64", F32, [128, 8])
    S.op("sp", lambda e: e.dma_start(out=g64[0:64, :], in_=g64_d), w=["g64"], dma=True)
    wst_a = A.alloc("wst_a", F32, [128, 8, 512])
    woa = [A.alloc("woa%d" % i, BF16, [128, 8, 512]) for i in range(2)]
    for dc in range(2):
        S.op("pool", lambda e, dc=dc: e.memset(woa[dc][64:128, :, :], 0.0), w=["woa%d" % dc])
        S.op("sp", lambda e, dc=dc: e.dma_start(out=wst_a[0:64, :, :], in_=w_out_d[0:512, dc * 512:(dc + 1) * 512].rearrange("(h p) f -> p h f", p=64)),
             w=["wst_a"], dma=True)
        S.op("pool", lambda e, dc=dc: e.tensor_tensor(out=woa[dc][0:64, :, :], in0=wst_a[0:64, :, :], in1=g64[0:64, :].unsqueeze(2).broadcast_to([64, 8, 512]), op=ALU.mult),
             r=["wst_a", "g64"], w=["woa%d" % dc])

    def att_wout(qc):
        for t in range(4 * qc, 4 * qc + 4):
            for dc in range(2):
                b = 6
                for h in range(8):
                    S.op("pe", lambda e, t=t, dc=dc, h=h, b=b: e.matmul(PS[:, b, :], lhsT=YA[:, h, t * 128:(t + 1) * 128], rhs=woa[dc][:, h, :], start=(h == 0), stop=(h == 7)),
                         r=[("YA", qc, h), "YA", "woa%d" % dc], w=[psk(b)])
                S.op("dve", lambda e, t=t, dc=dc, b=b: e.tensor_tensor(out=X[:, t, dc * 512:(dc + 1) * 512], in0=PS[:, b, :], in1=X[:, t, dc * 512:(dc + 1) * 512], op=ALU.add),
                     r=[psk(b), ("X", t)], w=[("X", t)])

    def att_scores(i):
        qc, j, kv, kt = its[i]
        b = SBANK[i % 4]
        S.op("pe", lambda e: e.matmul(PS[:, b, :], lhsT=KT[:, kv, kt * 128:(kt + 1) * 128],
                                      rhs=QT[:, j, qc * 512:(qc + 1) * 512], start=True, stop=True),
             r=[("KT", kt, kv)] + [("QT", t) for t in range(4 * qc, 4 * qc + 4)], w=[psk(b)])
        S.op("act", lambda e: e.activation(out=PT[i % 5], in_=PS[:, b, :], func=AF.Exp), r=[psk(b)], w=["PT%d" % (i % 5)])

    def att_finish(hi, qc, h, ob):
        sr, srk = SR[hi % 2], "SR%d" % (hi % 2)
        ri, rik = RI[hi % 2], "RI%d" % (hi % 2)
        nb = 4
        S.op("dve", lambda e: e.tensor_copy(out=sr[64:65, :], in_=PS[64:65, ob, :]), r=[psk(ob)], w=[srk])
        S.op("pe", lambda e: e.matmul(PS[0:64, nb, :], lhsT=onesf, rhs=sr, start=True, stop=True), r=["onesf", srk], w=[psk(nb)])
        S.op("dve", lambda e: e.reciprocal(out=ri[0:64, :], in_=PS[0:64, nb, :]), r=[psk(nb)], w=[rik])
        S.op("dve", lambda e: e.tensor_tensor(out=YA[0:64, h, qc * 512:(qc + 1) * 512], in0=PS[0:64, ob, :], in1=ri[0:64, :], op=ALU.mult),
             r=[psk(ob), rik], w=[("YA", qc, h)])

    sqa = A.alloc("sqa", BF16, [128, 8, 512])

    def att_norm(qc):
        yk = [("YA", qc, h) for h in range(8)]
        ri, rik = RI[0], "RI0"
        S.op("act", lambda e: e.activation(out=sqa[0:64, :, :], in_=YA[0:64, :, qc * 512:(qc + 1) * 512], func=AF.Square), r=yk, w=["sqa"])
        for h in range(8):
            S.op("pe", lambda e, h=h: e.matmul(PS[0:64, 6, :], lhsT=onesb[0:64, 0:64], rhs=sqa[0:64, h, :], start=(h == 0), stop=(h == 7)), r=["onesb", "sqa"], w=[psk(6)])
        S.op("act", lambda e: e.activation(out=ri[0:64, :], in_=PS[0:64, 6, :], func=AF.Sqrt, scale=1.0 / 512, bias=EPS), r=[psk(6)], w=[rik])
        S.op("dve", lambda e: e.reciprocal(out=ri[0:64, :], in_=ri[0:64, :]), r=[rik], w=[rik])
        S.op("dve", lambda e: e.tensor_tensor(out=YA[0:64, :, qc * 512:(qc + 1) * 512], in0=YA[0:64, :, qc * 512:(qc + 1) * 512],
                                              in1=ri[0:64, :].unsqueeze(1).broadcast_to([64, 8, 512]), op=ALU.mult), r=yk + [rik], w=yk)

    def att_pv(i):
        qc, j, kv, kt = its[i]
        hi = i // 16
        ob = 2 + (hi % 2)
        S.op("pe", lambda e: e.matmul(PS[:, ob, :], lhsT=VA[:, kt, kv, :], rhs=PT[i % 5], start=(kt == 0), stop=(kt == 15)),
             r=["PT%d" % (i % 5), ("VA", kt)], w=[psk(ob)])
        if kt == 15:
            h = kv * 4 + j
            deferred.append((i + 3, lambda: att_finish(hi, qc, h, ob)))
            if h == 7:
                deferred.append((i + 4, lambda: att_norm(qc)))
                deferred.append((i + 8, lambda: att_wout(qc)))

    def run_deferred(i):
        while deferred and deferred[0][0] <= i:
            deferred.pop(0)[1]()

    att_scores(0)
    att_scores(1)
    att_scores(2)
    att_scores(3)
    for i in range(len(its)):
        att_pv(i)
        if i + 4 < len(its):
            att_scores(i + 4)
        run_deferred(i)
    run_deferred(10 ** 9)
    A.free("QT", "KT", "VA", "PT0", "PT1", "PT2", "PT3", "PT4", "SR0", "SR1", "RI0", "RI1", "sqa", "onesf")

    if dbg == "att":
        A.free("wst_a", "woa0", "woa1")
        return dbg_finish([(YA[:, h, :], [("YA", qc, h) for qc in range(4)]) for h in range(8)])
    A.free("YA", "g64", "wst_a", "woa0", "woa1")
    s5raw = A.alloc("s5raw", F32, [128, 128])
    S.op("sp", lambda e: e.dma_start(out=s5raw[0:96, :], in_=s5p_d), w=["s5raw"], dma=True)
    S.op("pe", lambda e: e.transpose(out=PS[:, 7, 0:96], in_=s5raw[0:96, :], identity=idf[0:96, 0:96]), r=["s5raw", "idf"], w=[psk(7)])
    P5 = A.alloc("P5", F32, [128, 24, 32])
    NV = {"i": 3}

    def pv_(i):
        return P5[:, i, :]

    S.op("dve", lambda e: e.tensor_copy(out=P5[:, 0:3, :], in_=PS[:, 7, 0:96].rearrange("p (a b) -> p a b", a=3)), r=[psk(7)], w=["P5"])
    A.free("s5raw")
    LR, LI, LS, DT, MAG, ANG, KK, R1, SN, CS, ABR, ABI, DEN, NR, FR, FI, T0, T1, NABI = range(19)

    def v1(eng, fn):
        S.op(eng, fn, r=["P5"], w=["P5"])

    MAGIC = 12582912.0
    C1 = 6.28125
    C2 = float(2 * np.pi - 6.28125)
    v1("dve", lambda e: e.tensor_scalar(out=pv_(LR), in0=pv_(LR), scalar1=-1e-4, scalar2=None, op0=ALU.min))
    v1("act", lambda e: e.activation(out=pv_(DT), in_=pv_(LS), func=AF.Exp))
    v1("dve", lambda e: e.tensor_tensor(out=pv_(T0), in0=pv_(LR), in1=pv_(DT), op=ALU.mult))
    v1("act", lambda e: e.activation(out=pv_(MAG), in_=pv_(T0), func=AF.Exp))
    v1("dve", lambda e: e.tensor_tensor(out=pv_(ANG), in0=pv_(LI), in1=pv_(DT), op=ALU.mult))

    def sin_of(dst, shift):
        v1("dve", lambda e: e.tensor_scalar(out=pv_(T0), in0=pv_(ANG), scalar1=float(shift), scalar2=None, op0=ALU.add))
        v1("dve", lambda e: e.tensor_scalar(out=pv_(KK), in0=pv_(T0), scalar1=float(1 / (2 * np.pi)), scalar2=MAGIC, op0=ALU.mult, op1=ALU.add))
        v1("dve", lambda e: e.tensor_scalar(out=pv_(T1), in0=pv_(KK), scalar1=-MAGIC, scalar2=None, op0=ALU.add))
        v1("dve", lambda e: e.scalar_tensor_tensor(out=pv_(R1), in0=pv_(T1), scalar=-C1, in1=pv_(T0), op0=ALU.mult, op1=ALU.add))
        v1("dve", lambda e: e.scalar_tensor_tensor(out=pv_(KK), in0=pv_(T1), scalar=-C2, in1=pv_(R1), op0=ALU.mult, op1=ALU.add))
        v1("dve", lambda e: e.tensor_scalar(out=pv_(KK), in0=pv_(KK), scalar1=3.1415925, scalar2=-3.1415925, op0=ALU.min, op1=ALU.max))
        v1("act", lambda e: e.activation(out=pv_(dst), in_=pv_(KK), func=AF.Sin))

    sin_of(SN, 0.0)
    sin_of(CS, np.pi / 2)
    v1("dve", lambda e: e.tensor_tensor(out=pv_(ABR), in0=pv_(MAG), in1=pv_(CS), op=ALU.mult))
    v1("dve", lambda e: e.tensor_tensor(out=pv_(ABI), in0=pv_(MAG), in1=pv_(SN), op=ALU.mult))
    v1("dve", lambda e: e.tensor_scalar(out=pv_(NABI), in0=pv_(ABI), scalar1=-1.0, scalar2=None, op0=ALU.mult))
    v1("dve", lambda e: e.tensor_tensor(out=pv_(DEN), in0=pv_(LR), in1=pv_(LR), op=ALU.mult))
    v1("dve", lambda e: e.tensor_tensor(out=pv_(T0), in0=pv_(LI), in1=pv_(LI), op=ALU.mult))
    v1("dve", lambda e: e.tensor_tensor(out=pv_(DEN), in0=pv_(DEN), in1=pv_(T0), op=ALU.add))
    v1("dve", lambda e: e.reciprocal(out=pv_(DEN), in_=pv_(DEN)))
    v1("dve", lambda e: e.tensor_scalar(out=pv_(NR), in0=pv_(ABR), scalar1=-1.0, scalar2=None, op0=ALU.add))
    v1("dve", lambda e: e.tensor_tensor(out=pv_(T0), in0=pv_(NR), in1=pv_(LR), op=ALU.mult))
    v1("dve", lambda e: e.tensor_tensor(out=pv_(T1), in0=pv_(ABI), in1=pv_(LI), op=ALU.mult))
    v1("dve", lambda e: e.tensor_tensor(out=pv_(T0), in0=pv_(T0), in1=pv_(T1), op=ALU.add))
    v1("dve", lambda e: e.tensor_tensor(out=pv_(FR), in0=pv_(T0), in1=pv_(DEN), op=ALU.mult))
    v1("dve", lambda e: e.tensor_tensor(out=pv_(T0), in0=pv_(ABI), in1=pv_(LR), op=ALU.mult))
    v1("dve", lambda e: e.tensor_tensor(out=pv_(T1), in0=pv_(NR), in1=pv_(LI), op=ALU.mult))
    v1("dve", lambda e: e.tensor_tensor(out=pv_(T0), in0=pv_(T0), in1=pv_(T1), op=ALU.subtract))
    v1("dve", lambda e: e.tensor_tensor(out=pv_(FI), in0=pv_(T0), in1=pv_(DEN), op=ALU.mult))

    A16R, A16I = 19, 20
    S.op("dve", lambda e: e.tensor_copy(out=pv_(A16R), in_=pv_(ABR)), r=["P5"], w=["P5"])
    S.op("dve", lambda e: e.tensor_copy(out=pv_(A16I), in_=pv_(ABI)), r=["P5"], w=["P5"])
    for _ in range(3):
        v1("dve", lambda e: e.tensor_tensor(out=pv_(T0), in0=pv_(A16R), in1=pv_(A16R), op=ALU.mult))
        v1("dve", lambda e: e.tensor_tensor(out=pv_(T1), in0=pv_(A16I), in1=pv_(A16I), op=ALU.mult))
        v1("dve", lambda e: e.tensor_tensor(out=pv_(KK), in0=pv_(A16R), in1=pv_(A16I), op=ALU.mult))
        v1("dve", lambda e: e.tensor_tensor(out=pv_(A16R), in0=pv_(T0), in1=pv_(T1), op=ALU.subtract))
        v1("dve", lambda e: e.tensor_scalar(out=pv_(A16I), in0=pv_(KK), scalar1=2.0, scalar2=None, op0=ALU.mult))
    PW2R = A.alloc("PW2R", F32, [128, 32, 16])
    PW2I = A.alloc("PW2I", F32, [128, 32, 16])
    PWt = A.alloc("PWt", F32, [128, 2, 32, 8])
    kp2 = ["PW2R", "PW2I", "PWt", "P5"]
    S.op("dve", lambda e: e.tensor_copy(out=PW2R[:, :, 0], in_=pv_(A16R)), r=kp2, w=kp2)
    S.op("dve", lambda e: e.tensor_copy(out=PW2I[:, :, 0], in_=pv_(A16I)), r=kp2, w=kp2)
    m_ = 1
    while m_ < 16:
        br_ = PW2R[:, :, m_ - 1].unsqueeze(2).broadcast_to([128, 32, m_])
        bi_ = PW2I[:, :, m_ - 1].unsqueeze(2).broadcast_to([128, 32, m_])
        t0_, t1_ = PWt[:, 0, :, 0:m_], PWt[:, 1, :, 0:m_]
        pr_, pi2_ = PW2R[:, :, 0:m_], PW2I[:, :, 0:m_]
        S.op("dve", lambda e, t0_=t0_, pr_=pr_, br_=br_: e.tensor_tensor(out=t0_, in0=pr_, in1=br_, op=ALU.mult), r=kp2, w=kp2)
        S.op("dve", lambda e, t1_=t1_, pi2_=pi2_, bi_=bi_: e.tensor_tensor(out=t1_, in0=pi2_, in1=bi_, op=ALU.mult), r=kp2, w=kp2)
        S.op("dve", lambda e, m_=m_, t0_=t0_, t1_=t1_: e.tensor_tensor(out=PW2R[:, :, m_:2 * m_], in0=t0_, in1=t1_, op=ALU.subtract), r=kp2, w=kp2)
        S.op("dve", lambda e, t0_=t0_, pr_=pr_, bi_=bi_: e.tensor_tensor(out=t0_, in0=pr_, in1=bi_, op=ALU.mult), r=kp2, w=kp2)
        S.op("dve", lambda e, t1_=t1_, pi2_=pi2_, br_=br_: e.tensor_tensor(out=t1_, in0=pi2_, in1=br_, op=ALU.mult), r=kp2, w=kp2)
        S.op("dve", lambda e, m_=m_, t0_=t0_, t1_=t1_: e.tensor_tensor(out=PW2I[:, :, m_:2 * m_], in0=t0_, in1=t1_, op=ALU.add), r=kp2, w=kp2)
        m_ *= 2
    A.free("PWt")

    masksB = A.alloc("masksB", F32, [128, 4, 128])
    S.op("sp", lambda e: e.dma_start(out=masksB, in_=masksB_d), w=["masksB"], dma=True)
    masks = A.alloc("masks", F32, [128, 2, 2, 128])
    S.op("sp", lambda e: e.dma_start(out=masks[:, 0, :, :], in_=masks_d[0]), w=["masks"], dma=True)
    S.op("sp", lambda e: e.dma_start(out=masks[:, 1, :, :], in_=masks_d[1]), w=["masks"], dma=True)
    Bre = A.alloc("Bre", F32, [128, 32, 16])
    Bim = A.alloc("Bim", F32, [128, 32, 16])
    BBr = A.alloc("BBr", F32, [128, 32, 16])
    BBi = A.alloc("BBi", F32, [128, 32, 16])
    Bt = A.alloc("Bt", F32, [128, 32, 16])
    for dd in range(2):
        S.op("sp", lambda e, dd=dd: e.dma_start(out=Bre[:, 16 * dd:16 * dd + 16, :], in_=bre_d[dd].rearrange("(gp gl) p c -> (gl p) gp c", gl=2)),
             w=["Bre"], dma=True)
        S.op("sp", lambda e, dd=dd: e.dma_start(out=Bim[:, 16 * dd:16 * dd + 16, :], in_=bim_d[dd].rearrange("(gp gl) p c -> (gl p) gp c", gl=2)),
             w=["Bim"], dma=True)

    def bc(i):
        return pv_(i).unsqueeze(2).broadcast_to([128, 32, 16])

    S.op("dve", lambda e: e.tensor_tensor(out=BBr, in0=Bre, in1=bc(FR), op=ALU.mult), r=["Bre", "P5"], w=["BBr"])
    S.op("dve", lambda e: e.tensor_tensor(out=Bt, in0=Bim, in1=bc(FI), op=ALU.mult), r=["Bim", "P5"], w=["Bt"])
    S.op("dve", lambda e: e.tensor_tensor(out=BBr, in0=BBr, in1=Bt, op=ALU.subtract), r=["BBr", "Bt"], w=["BBr"])
    S.op("dve", lambda e: e.tensor_tensor(out=BBi, in0=Bim, in1=bc(FR), op=ALU.mult), r=["Bim", "P5"], w=["BBi"])
    S.op("dve", lambda e: e.tensor_tensor(out=Bt, in0=Bre, in1=bc(FI), op=ALU.mult), r=["Bre", "P5", "BBr"], w=["Bt"])
    S.op("dve", lambda e: e.tensor_tensor(out=BBi, in0=BBi, in1=Bt, op=ALU.add), r=["BBi", "Bt"], w=["BBi"])
    A.free("Bre", "Bim", "Bt")
    TB = A.alloc("TB", BF16, [128, 64, 128])
    TC = A.alloc("TC", BF16, [128, 32, 128])
    ZT = A.alloc("ZT", BF16, [128, 8, 128])
    for ri in range(2):
        BB = BBr if ri == 0 else BBi
        bbk = "BBr" if ri == 0 else "BBi"
        for m4 in range(4):
            for dfc in range(8):
                S.op("dve", lambda e, BB=BB, m4=m4, dfc=dfc: e.tensor_tensor(
                    out=ZT[:, dfc, :].rearrange("p (m g c) -> p m g c", m=4, g=2),
                    in0=BB[:, dfc * 4:(dfc + 1) * 4, :].unsqueeze(2).broadcast_to([128, 4, 2, 16]),
                    in1=masksB[:, m4, :].rearrange("p (m g c) -> p m g c", m=4, g=2), op=ALU.mult),
                    r=[bbk, "masksB"], w=[("ZT", dfc)])
            b = 4 + (ri * 4 + m4) % 2
            pvb = psbf(b)
            for dfc in range(8):
                S.op("pe", lambda e, dfc=dfc, pvb=pvb: e.transpose(out=pvb[:, dfc * 128:(dfc + 1) * 128], in_=ZT[:, dfc, :], identity=idb),
                     r=[("ZT", dfc), "idb"], w=[psk(b)])
            base = (ri * 4 + m4) * 8
            copy_op("dve", TB[:, base:base + 8, :], pvb.rearrange("p (a b) -> p a b", a=8), [psk(b)], [("TB", ri, m4)])
    A.free("BBr", "BBi")
    Cn = [A.alloc("Cn%d" % i, F32, [128, 8, 64]) for i in range(2)]
    for ri, cd in enumerate([cre_d, cim_d]):
        for dd in range(2):
            S.op("sp", lambda e, ri=ri, cd=cd, dd=dd: e.dma_start(out=Cn[ri][:, 4 * dd:4 * dd + 4, :], in_=cd[dd].rearrange("(fc g8) co p -> (g8 co) fc p", g8=8)),
                 w=["Cn%d" % ri], dma=True)
    for ri in range(2):
        for par in range(2):
            for dfc in range(8):
                if ri == 0:
                    S.op("dve", lambda e, par=par, dfc=dfc: e.tensor_tensor(
                        out=ZT[:, dfc, :].rearrange("p (g q) -> p g q", g=2),
                        in0=Cn[0][:, dfc, :].unsqueeze(1).broadcast_to([128, 2, 64]),
                        in1=masks[:, 1, par, :].rearrange("p (g q) -> p g q", g=2), op=ALU.mult),
                        r=["Cn0", "masks"], w=[("ZT", dfc)])
                else:
                    S.op("dve", lambda e, par=par, dfc=dfc: e.scalar_tensor_tensor(
                        out=ZT[:, dfc, :].rearrange("p (g q) -> p g q", g=2),
                        in0=Cn[1][:, dfc, :].unsqueeze(1).broadcast_to([128, 2, 64]), scalar=-1.0,
                        in1=masks[:, 1, par, :].rearrange("p (g q) -> p g q", g=2), op0=ALU.mult, op1=ALU.mult),
                        r=["Cn1", "masks"], w=[("ZT", dfc)])
            b = 4 + (ri * 2 + par) % 2
            pvb = psbf(b)
            for dfc in range(8):
                S.op("pe", lambda e, dfc=dfc, pvb=pvb: e.transpose(out=pvb[:, dfc * 128:(dfc + 1) * 128], in_=ZT[:, dfc, :], identity=idb),
                     r=[("ZT", dfc), "idb"], w=[psk(b)])
            base = (ri * 2 + par) * 8
            copy_op("dve", TC[:, base:base + 8, :], pvb.rearrange("p (a b) -> p a b", a=8), [psk(b)], [("TC", ri, par)])
    if dbg == "s5tb":
        return dbg_finish([(TB[:, 0:8, :].rearrange("p a b -> p (a b)"), [("TB", 0, 0)]), (TB[:, 16:24, :].rearrange("p a b -> p (a b)"), [("TB", 1, 0)]),
                           (P5.rearrange("p a b -> p (a b)"), ["P5"])])
    A.free("Cn0", "Cn1", "ZT", "masks", "masksB")

    YS = A.alloc("YS", F32, [128, 4, 2048], top=True)
    for fc in range(4):
        S.op("pool", lambda e, fc=fc: e.tensor_scalar(out=YS[:, fc, :], in0=UT[:, fc, :, :].rearrange("p j b -> p (j b)"), scalar1=gT[:, 40 + fc:41 + fc], scalar2=0.0, op0=ALU.mult, op1=ALU.add),
             r=[("UT", fc, tc) for tc in range(4)] + ["gT"], w=[("YS", fc, tc) for tc in range(4)])

    LB_ = 8
    NBLK = 2048 // LB_
    NQ = 2
    Sst = [A.alloc("Sst%d" % q, BF16, [128, 2, LB_, NBLK]) for q in range(NQ)]
    DG = [A.alloc("DG%d" % q, BF16, [128, 3, 128]) for q in range(2 * NQ)]
    Hb = [A.alloc("Hb%d" % q, BF16, [128, 2, NBLK]) for q in range(NQ)]
    FPs_ = [A.alloc("FPall%d" % g, F32, [128, 8, 2, NBLK]) for g in range(2)]
    bcount = {"n": 0}
    EV = [A.alloc("EV%d" % i, F32, [128, 512]) for i in range(2)]
    TTm = A.alloc("TTm", F32, [128, 4, 128])

    def tt(out, in0, in1, op, r, w):
        S.op("dve", lambda e: e.tensor_tensor(out=out, in0=in0, in1=in1, op=op), r=r, w=w)

    def bstep(dst, src, tab, tview, kd, ks, kx):
        T = [tview(q) for q in range(4)]
        tk = [("TTm", q) for q in range(4)]
        kx = list(kx)
        tt(T[0], src[0], tab[0], ALU.mult, [ks[0]] + kx, [tk[0]])
        tt(T[1], src[1], tab[1], ALU.mult, [ks[1]] + kx, [tk[1]])
        tt(T[2], src[1], tab[0], ALU.mult, [ks[1]] + kx, [tk[2]])
        tt(T[3], src[0], tab[1], ALU.mult, [ks[0]] + kx, [tk[3]])
        tt(dst[0], dst[0], T[0], ALU.add, [kd[0], tk[0]], [kd[0]])
        tt(dst[1], dst[1], T[2], ALU.add, [kd[1], tk[2]], [kd[1]])
        tt(dst[0], dst[0], T[1], ALU.subtract, [kd[0], tk[1]], [kd[0]])
        tt(dst[1], dst[1], T[3], ALU.add, [kd[1], tk[3]], [kd[1]])

    def batch_pass(d, items, final, FPall, fpn):
        fwd = (d == 0)
        info = []
        bpar = bcount["n"] % 2
        bcount["n"] += 1
        for q, (i, gp) in enumerate(items):
            c = d * 16 + gp
            fc, m = gp // 4, gp % 4
            h, par = m // 2, m % 2
            dfc = d * 4 + fc
            dq = bpar * NQ + q
            info.append((i, c, fc, m, h, par, dfc, dq))
            for k3, slot in enumerate([ABR, NABI, ABI]):
                S.op("pool", lambda e, dq=dq, k3=k3, slot=slot, c=c: e.tensor_scalar(out=DG[dq][:, k3, :], in0=idb, scalar1=P5[:, slot, c:c + 1], scalar2=0.0, op0=ALU.mult, op1=ALU.add),
                     r=["idb", "P5"], w=[("DG%d" % dq, k3)])
            if final:
                S.op("pool", lambda e, q=q: e.memset(Hb[q], 0.0), w=["Hb%d" % q])
                if fwd:
                    S.op("act", lambda e, q=q, i=i: e.copy(out=Hb[q][:, :, 1:NBLK], in_=FPall[:, i, :, 0:NBLK - 1]), r=[(fpn, i, 0), (fpn, i, 1), "Hb%d" % q], w=["Hb%d" % q])
                else:
                    S.op("act", lambda e, q=q, i=i: e.copy(out=Hb[q][:, :, 0:NBLK - 1], in_=FPall[:, i, :, 1:NBLK]), r=[(fpn, i, 0), (fpn, i, 1), "Hb%d" % q], w=["Hb%d" % q])
        js = list(range(LB_)) if fwd else list(range(LB_ - 1, -1, -1))
        for s_, j in enumerate(js):
            jp = js[s_ - 1] if s_ > 0 else None
            for q, (i, c, fc, m, h, par, dfc, dq) in enumerate(info):
                bank = 4 + bpar * NQ + q
                sk = "Sst%d" % q
                dgk = [("DG%d" % dq, k3) for k3 in range(3)]
                if s_ > 0:
                    prev = (Sst[q][:, 0, jp, :], Sst[q][:, 1, jp, :])
                    pk = [(sk, jp)]
                elif final:
                    prev = (Hb[q][:, 0, :], Hb[q][:, 1, :])
                    pk = ["Hb%d" % q]
                else:
                    prev = None
                    pk = []
                u_rhs = UT[:, fc, j, :]
                uk = [("UT", fc, tc) for tc in range(4)]
                last_bu = prev is None
                for ri in range(2):
                    tbi = (ri * 4 + m) * 8 + dfc
                    S.op("pe", lambda e, ri=ri, tbi=tbi, bank=bank, u_rhs=u_rhs, last_bu=last_bu: e.matmul(
                        PS[:, bank, ri * 256:(ri + 1) * 256], lhsT=TB[:, tbi, :], rhs=u_rhs, start=(ri == 0), stop=last_bu, skip_group_check=True),
                        r=[("TB", ri, m)] + uk, w=[psk(bank)])
                if prev is not None:
                    plan = [(0, 0, 0), (0, 1, 1), (1, 0, 1), (1, 2, 0)]
                    for n_, (ro, k3, rp) in enumerate(plan):
                        S.op("pe", lambda e, dq=dq, ro=ro, k3=k3, rp=rp, bank=bank, prev=prev, n_=n_: e.matmul(
                            PS[:, bank, ro * 256:(ro + 1) * 256], lhsT=DG[dq][:, k3, :], rhs=prev[rp], start=False, stop=(n_ % 2 == 1), skip_group_check=True),
                            r=dgk + pk, w=[psk(bank)])
                S.op("act", lambda e, q=q, j=j, bank=bank: e.copy(out=Sst[q][:, :, j, :], in_=PS[:, bank, :].rearrange("p (r b) -> p r b", r=2)),
                     r=[psk(bank)], w=[(sk, j)])
                if (not final) and s_ == LB_ - 1:
                    S.op("act", lambda e, i=i, bank=bank: e.copy(out=FPall[:, i, :, :], in_=PS[:, bank, :].rearrange("p (r b) -> p r b", r=2)),
                         r=[psk(bank)], w=[(fpn, i, 0), (fpn, i, 1)])
        if not final:
            return
        fc = info[0][2]
        for tc in range(4):
            for q, (i, c, fc_, m, h, par, dfc, dq) in enumerate(info):
                for ri in range(2):
                    tci = (ri * 2 + par) * 8 + dfc
                    S.op("pe", lambda e, q=q, tci=tci, tc=tc, ri=ri, h=h, par=par: e.matmul(
                        PS[64 * h:64 * h + 64, tc, :], lhsT=TC[:, tci, 64 * h:64 * h + 64],
                        rhs=Sst[q][:, ri, 2 * tc:2 * tc + 2, :].rearrange("p j b -> p (j b)"),
                        start=(par == 0 and ri == 0), stop=(par == 1 and ri == 1), skip_group_check=True),
                        r=[("TC", ri, par)] + [("Sst%d" % q, j) for j in range(LB_)], w=[psk(tc)])
            if info[-1][3] == 3:
                ev = EV[tc % 2]
                evk = "EV%d" % (tc % 2)
                S.op("act", lambda e, tc=tc, ev=ev: e.copy(out=ev, in_=PS[:, tc, :]), r=[psk(tc)], w=[evk])
                S.op("pool", lambda e, tc=tc, fc=fc, ev=ev: e.tensor_tensor(out=YS[:, fc, tc * 512:(tc + 1) * 512], in0=ev, in1=YS[:, fc, tc * 512:(tc + 1) * 512], op=ALU.add),
                     r=[evk, ("YS", fc, tc)], w=[("YS", fc, tc)])

    def solve_carries(d, c0, FPall, fpn):
        fwd = (d == 0)
        Fv = [FPall[:, :, ri, :].rearrange("p c (B k) -> p c B k", k=16) for ri in range(2)]
        fk = lambda k: (("FPs", 0, k), ("FPs", 1, k))
        allk = [(fpn, i, ri) for i in range(8) for ri in range(2)]
        S.op("dve", lambda e: e.tensor_copy(out=TTm[:, 0, 0:8], in_=P5[:, 0, 0:8]), r=allk + ["P5"], w=[("FPs", ri, k) for ri in range(2) for k in range(16)] + [("TTm", 0)])
        tv3 = lambda q: TTm[:, q, :].rearrange("p (c b) -> p c b", c=8)
        a8 = (P5[:, A16R, c0:c0 + 8].unsqueeze(2).broadcast_to([128, 8, 16]), P5[:, A16I, c0:c0 + 8].unsqueeze(2).broadcast_to([128, 8, 16]))
        for k in (range(1, 16) if fwd else range(14, -1, -1)):
            kp = k - 1 if fwd else k + 1
            bstep((Fv[0][:, :, :, k], Fv[1][:, :, :, k]), (Fv[0][:, :, :, kp], Fv[1][:, :, :, kp]), a8, tv3, fk(k), fk(kp), ["P5"])
        kl = 15 if fwd else 0
        a128 = (PW2R[:, c0:c0 + 8, 15], PW2I[:, c0:c0 + 8, 15])
        tv1 = lambda q: TTm[:, q, 0:8]
        for B in (range(1, 16) if fwd else range(14, -1, -1)):
            Bp = B - 1 if fwd else B + 1
            bstep((Fv[0][:, :, B, kl], Fv[1][:, :, B, kl]), (Fv[0][:, :, Bp, kl], Fv[1][:, :, Bp, kl]), a128, tv1, fk(kl), fk(kl), ["PW2R", "PW2I"])
        tv15 = lambda q: TTm[:, q, 0:120].rearrange("p (c b) -> p c b", c=8)
        for k in (range(0, 15) if fwd else range(1, 16)):
            pi_ = k if fwd else 15 - k
            tab = (PW2R[:, c0:c0 + 8, pi_].unsqueeze(2).broadcast_to([128, 8, 15]), PW2I[:, c0:c0 + 8, pi_].unsqueeze(2).broadcast_to([128, 8, 15]))
            if fwd:
                dst = (Fv[0][:, :, 1:16, k], Fv[1][:, :, 1:16, k])
                src = (Fv[0][:, :, 0:15, 15], Fv[1][:, :, 0:15, 15])
            else:
                dst = (Fv[0][:, :, 0:15, k], Fv[1][:, :, 0:15, k])
                src = (Fv[0][:, :, 1:16, 0], Fv[1][:, :, 1:16, 0])
            bstep(dst, src, tab, tv15, fk(k), fk(kl), ["PW2R", "PW2I"])
        S.op("dve", lambda e: e.tensor_copy(out=TTm[:, 0, 0:8], in_=P5[:, 0, 0:8]), r=[("FPs", ri, k) for ri in range(2) for k in range(16)] + ["P5"], w=allk + [("TTm", 0)])

    groups = [(d, fp) for d in range(2) for fp in range(2)]

    def run_pass(g, final):
        d, fp = groups[g]
        gps = [8 * fp + i for i in range(8)]
        for b0 in range(0, 8, NQ):
            batch_pass(d, [(i, gps[i]) for i in range(b0, b0 + NQ)], final, FPs_[g % 2], "FPall%d" % (g % 2))

    def do_solve(g):
        d, fp = groups[g]
        solve_carries(d, d * 16 + 8 * fp, FPs_[g % 2], "FPall%d" % (g % 2))

    run_pass(0, False)
    if dbg == "s5f":
        return dbg_finish([(FPs_[0].rearrange("p a b c -> p (a b c)"), [("FPall0", i, ri) for i in range(8) for ri in range(2)])])
    run_pass(1, False)
    do_solve(0)
    if dbg == "s5c":
        return dbg_finish([(FPs_[0].rearrange("p a b c -> p (a b c)"), [("FPall0", i, ri) for i in range(8) for ri in range(2)])])
    do_solve(1)
    run_pass(0, True)
    run_pass(2, False)
    do_solve(2)
    run_pass(1, True)
    run_pass(3, False)
    do_solve(3)
    run_pass(2, True)
    run_pass(3, True)
    A.free("EV0", "EV1", "Sst0", "Sst1", "DG0", "DG1", "DG2", "DG3", "Hb0", "Hb1", "FPall0", "FPall1", "TTm", "TB", "TC", "PW2R", "PW2I", "P5")
    GLUW = A.alloc("GLUW", BF16, [128, 4, 512])
    gst = A.alloc("gst", F32, [128, 4, 512])
    S.op("sp", lambda e: e.dma_start(out=gst, in_=glu_w_d.rearrange("(k p) f -> p k f", p=128)), w=["gst"], dma=True)
    S.op("pool", lambda e: e.tensor_scalar(out=GLUW, in0=gst, scalar1=1.0, scalar2=0.0, op0=ALU.mult, op1=ALU.add), r=["gst"], w=["GLUW"])
    A.free("gst")

    if dbg == "s5":
        dbg_d = nc.dram_tensor("dbg", [128, 8192], F32, kind="ExternalOutput").ap()
        S.op("sp", lambda e: e.dma_start(out=dbg_d, in_=YS.rearrange("p a b -> p (a b)")), r=[("YS", a, b) for a in range(4) for b in range(4)], w=["o1"], dma=True)
        S.op("sp", lambda e: e.nop(), r=["o1"])
        S.emit(nc)
        es.close()
        return nc

    A.free("UT")
    Z2 = A.alloc("Z2", BF16, [128, 4, 2048])
    gt = [A.alloc("gt%d" % i, F32, [128, 512]) for i in range(4)]
    for tc in range(4):
        for fc in range(4):
            i = (tc * 4 + fc) % 4
            g_ = gt[i]
            gk_ = "gt%d" % i
            xs = YS[:, fc, tc * 512:(tc + 1) * 512]
            yk = ("YS", fc, tc)
            S.op("act", lambda e, g_=g_, xs=xs: e.activation(out=g_, in_=xs, func=AF.Square), r=[yk], w=[gk_])
            S.op("dve", lambda e, g_=g_: e.tensor_scalar(out=g_, in0=g_, scalar1=0.044715, scalar2=1.0, op0=ALU.mult, op1=ALU.add), r=[gk_], w=[gk_])
            S.op("dve", lambda e, g_=g_, xs=xs: e.tensor_tensor(out=g_, in0=g_, in1=xs, op=ALU.mult), r=[gk_, yk], w=[gk_])
            S.op("act", lambda e, g_=g_: e.activation(out=g_, in_=g_, func=AF.Tanh, scale=0.7978845608028654), r=[gk_], w=[gk_])
            S.op("dve", lambda e, g_=g_, xs=xs, fc=fc, tc=tc: e.scalar_tensor_tensor(out=Z2[:, fc, tc * 512:(tc + 1) * 512], in0=g_, scalar=1.0, in1=xs, op0=ALU.add, op1=ALU.mult),
                 r=[gk_, yk], w=[("Z2", fc, tc)])
    if dbg == "gelu":
        return dbg_finish([(Z2[:, 1, :], [("Z2", 1, tc) for tc in range(4)])])
    for tc in range(4):
        for fo in range(4):
            i = (tc * 4 + fo) % 4
            b = 4 + i
            g_ = gt[i]
            gk_ = "gt%d" % i
            for fk in range(4):
                S.op("pe", lambda e, fo=fo, tc=tc, fk=fk, b=b: e.matmul(PS[:, b, :], lhsT=GLUW[:, fk, fo * 128:(fo + 1) * 128], rhs=Z2[:, fk, tc * 512:(tc + 1) * 512],
                                                                        start=(fk == 0), stop=(fk == 3)),
                     r=["GLUW"] + [("Z2", fk, tc)], w=[psk(b)])
            S.op("act", lambda e, g_=g_, fo=fo, b=b: e.activation(out=g_, in_=PS[:, b, :], func=AF.Sigmoid, scale=0.5, bias=gT[:, 44 + fo:45 + fo]),
                 r=[psk(b), "gT"], w=[gk_])
            S.op("dve", lambda e, g_=g_, fo=fo, tc=tc: e.scalar_tensor_tensor(out=YS[:, fo, tc * 512:(tc + 1) * 512], in0=Z2[:, fo, tc * 512:(tc + 1) * 512], scalar=0.5, in1=g_,
                                                                               op0=ALU.mult, op1=ALU.mult),
                 r=[gk_, ("Z2", fo, tc)] + [("YS", f2, tc) for f2 in range(4)], w=[("YS", fo, tc)])
    if dbg == "glu":
        return dbg_finish([(YS[:, 1, :], [("YS", 1, tc) for tc in range(4)])])
    YTs = A.alloc("YTs", BF16, [128, 4, 2048], top=True)
    sqb = A.alloc("sqb", BF16, [128, 4, 512])
    for tc in range(4):
        b = 6 + (tc % 2)
        g_ = gt[tc % 2]
        gk_ = "gt%d" % (tc % 2)
        S.op("act", lambda e, tc=tc: e.activation(out=sqb, in_=YS[:, :, tc * 512:(tc + 1) * 512], func=AF.Square),
             r=[("YS", f2, tc) for f2 in range(4)], w=["sqb"])
        for fk in range(4):
            S.op("pe", lambda e, fk=fk, b=b: e.matmul(PS[:, b, :], lhsT=onesb, rhs=sqb[:, fk, :], start=(fk == 0), stop=(fk == 3)), r=["onesb", "sqb"], w=[psk(b)])
        S.op("act", lambda e, g_=g_, b=b: e.activation(out=g_, in_=PS[:, b, :], func=AF.Sqrt, scale=1.0 / 512, bias=EPS), r=[psk(b)], w=[gk_])
        S.op("dve", lambda e, g_=g_: e.reciprocal(out=g_, in_=g_), r=[gk_], w=[gk_])
        for jj in range(2):
            S.op("dve", lambda e, g_=g_, tc=tc, jj=jj: e.tensor_tensor(out=YTs[:, :, 2 * tc + jj:2048:8], in0=YS[:, :, tc * 512 + jj * 256:tc * 512 + (jj + 1) * 256],
                                                                in1=g_[:, jj * 256:(jj + 1) * 256].unsqueeze(1).broadcast_to([128, 4, 256]), op=ALU.mult),
                 r=[gk_] + [("YS", f2, tc) for f2 in range(4)], w=[("YTs", tc, jj)])
    if dbg == "ssmn":
        return dbg_finish([(YTs[:, 1, :], [("YTs", tc, jj) for tc in range(4) for jj in range(2)])])
    A.free("YS", "Z2", "gt0", "gt1", "gt2", "gt3", "sqb", "GLUW")

    w_open(2, 4)
    wo_ = [load_w(w_out_d[512:1024, dc * 512:(dc + 1) * 512].rearrange("(k p) f -> p k f", p=128), 4, 512, gcol=36) for dc in range(2)]
    for t in range(16):
        for dc in range(2):
            b = (t * 2 + dc) % 4
            for k in range(4):
                S.op("pe", lambda e, t=t, dc=dc, k=k, b=b: e.matmul(PS[:, b, :], lhsT=YTs[:, k, t * 128:(t + 1) * 128], rhs=wo_[dc][0][:, k, :], start=(k == 0), stop=(k == 3)),
                     r=[("YTs", a_, b_) for a_ in range(4) for b_ in range(2)] + [wo_[dc][1]], w=[psk(b)])
            S.op("dve", lambda e, t=t, dc=dc, b=b: e.tensor_tensor(out=X[:, t, dc * 512:(dc + 1) * 512], in0=PS[:, b, :], in1=X[:, t, dc * 512:(dc + 1) * 512], op=ALU.add),
                 r=[psk(b), ("X", t)], w=[("X", t)])
    A.free("YTs")
    if dbg == "wout":
        S.op("sp", lambda e: e.dma_start(out=out_v, in_=X), r=[("X", t) for t in range(16)], w=["o1"], dma=True)
        S.op("sp", lambda e: e.nop(), r=["o1"])
        S.emit(nc)
        es.close()
        return nc

    M_ = A.alloc("M_", F32, [128, 2, 1024])
    S.op("sp", lambda e: e.dma_start(out=M_, in_=mem_d.rearrange("(n p) d -> p n d", p=128)), w=["M_"], dma=True)

    def mtile(t):
        return M_[:, t, :], ["M_"]

    hmT = A.alloc("hmT", BF16, [128, 8, 256])
    rstdm = rms_stats(mtile, 2, 1024, "rstdm")
    norm_transpose(mtile, 2, 8, rstdm, "rstdm", hmT, "hmT", [4, 5], "nm")
    A.free("rstdm", "M_")
    KX = A.alloc("KX", BF16, [128, 8, 256])
    VX = A.alloc("VX", BF16, [128, 2, 1024])
    for half in range(2):
        wb, wk_ = load_w(wk_d[:, half * 512:(half + 1) * 512].rearrange("(k p) f -> p k f", p=128), 8, 512, gcol=16)
        for i in range(4):
            b = 4 + (i % 2)
            for k in range(8):
                S.op("pe", lambda e, wb=wb, i=i, k=k, b=b: e.matmul(PS[:, b, 0:256], lhsT=wb[:, k, i * 128:(i + 1) * 128], rhs=hmT[:, k, :], start=(k == 0), stop=(k == 7)),
                     r=[wk_, ("hmT", 0), ("hmT", 1)], w=[psk(b)])
            copy_op(alt(), KX[:, half * 4 + i, :], PS[:, b, 0:256], [psk(b)], [("KX", half * 4 + i)])
    for half in range(2):
        wb, wk_ = load_w(wv_d[:, half * 512:(half + 1) * 512].rearrange("(k p) f -> p k f", p=128), 8, 512, gcol=16)
        for mt in range(2):
            b = 6 + (mt % 2)
            for k in range(8):
                S.op("pe", lambda e, wb=wb, mt=mt, k=k, b=b: e.matmul(PS[:, b, :], lhsT=hmT[:, k, mt * 128:(mt + 1) * 128], rhs=wb[:, k, :], start=(k == 0), stop=(k == 7)),
                     r=[wk_, ("hmT", mt)], w=[psk(b)])
            copy_op(alt(), VX[:, mt, half * 512:(half + 1) * 512], PS[:, b, :], [psk(b)], [("VX", mt, half)])
    A.free("hmT")
    HT = A.alloc("HTx", BF16, [128, 8, 2048], top=True)
    rstd3 = rms_stats(xtile, 16, 1024, "rstd3")
    norm_transpose(xtile, 16, 8, rstd3, "rstd3", HT, "HTx", [0, 1], "n3")
    A.free("rstd3")
    wq_ = [load_w(wq_d[:, hf * 512:(hf + 1) * 512].rearrange("(k p) f -> p k f", p=128), 8, 512, gcol=8) for hf in range(2)]
    wox = [load_w(wo_d[:, hf * 512:(hf + 1) * 512].rearrange("(k p) f -> p k f", p=128), 8, 512) for hf in range(2)]
    A.free(*wstate["STn"])
    wstate["STn"] = []
    QXs = [A.alloc("QX%d" % i, BF16, [128, 8, 512]) for i in range(2)]
    PX = [A.alloc("PX%d" % i, BF16, [128, 512]) for i in range(4)]
    OT = A.alloc("OT", BF16, [128, 8, 512])
    rsxs = [A.alloc("rsx%d" % i, F32, [128, 512]) for i in range(2)]

    def xa_q(tc):
        QX, qn_ = QXs[tc % 2], "QX%d" % (tc % 2)
        htk = [("HTx", t) for t in range(4 * tc, 4 * tc + 4)]
        for f in range(8):
            b = f % 2
            for k in range(8):
                S.op("pe", lambda e, f=f, k=k, b=b: e.matmul(PS[:, b, :], lhsT=wq_[f // 4][0][:, k, (f % 4) * 128:(f % 4 + 1) * 128], rhs=HT[:, k, tc * 512:(tc + 1) * 512],
                                                             start=(k == 0), stop=(k == 7)),
                     r=htk + [wq_[f // 4][1]], w=[psk(b)])
            copy_op(alt(), QX[:, f, :], PS[:, b, :], [psk(b)], [(qn_, f)])

    def xa_heads(tc):
        for hh in range(4):
            xa_head(tc, hh)

    def xa_head(tc, hh):
        QX, qn_ = QXs[tc % 2], "QX%d" % (tc % 2)
        rsx, rk = rsxs[hh % 2], "rsx%d" % (hh % 2)
        sb_ = 4 if hh % 2 == 0 else 7
        for mt in range(2):
            b = 2 + mt
            pi = (hh % 2) * 2 + mt
            for kk in range(2):
                S.op("pe", lambda e, mt=mt, kk=kk, b=b: e.matmul(PS[:, b, :], lhsT=KX[:, 2 * hh + kk, mt * 128:(mt + 1) * 128], rhs=QX[:, 2 * hh + kk, :],
                                                                 start=(kk == 0), stop=(kk == 1)),
                     r=[("KX", 2 * hh + kk), (qn_, 2 * hh + kk)], w=[psk(b)])
            S.op("act", lambda e, pi=pi, b=b: e.activation(out=PX[pi], in_=PS[:, b, :], func=AF.Exp, scale=1.0 / 16), r=[psk(b)], w=["PX%d" % pi])
        for mt in range(2):
            pi = (hh % 2) * 2 + mt
            S.op("pe", lambda e, mt=mt, pi=pi: e.matmul(PS[:, sb_, :], lhsT=onesb, rhs=PX[pi], start=(mt == 0), stop=(mt == 1)), r=["onesb", "PX%d" % pi], w=[psk(sb_)])
        S.op("dve", lambda e: e.reciprocal(out=rsx, in_=PS[:, sb_, :]), r=[psk(sb_)], w=[rk])
        for ff in range(2):
            b = 5 + ff
            f = 2 * hh + ff
            for mt in range(2):
                pi = (hh % 2) * 2 + mt
                S.op("pe", lambda e, mt=mt, f=f, b=b, pi=pi: e.matmul(PS[:, b, :], lhsT=VX[:, mt, f * 128:(f + 1) * 128], rhs=PX[pi], start=(mt == 0), stop=(mt == 1)),
                     r=[("VX", mt, f // 4), "PX%d" % pi], w=[psk(b)])
            S.op("dve", lambda e, f=f, b=b: e.tensor_tensor(out=OT[:, f, :], in0=PS[:, b, :], in1=rsx, op=ALU.mult), r=[psk(b), rk], w=[("OT", f)])

    def xa_o(tc):
        for tt in range(4):
            t = tc * 4 + tt
            for dc in range(2):
                b = dc
                for k in range(8):
                    S.op("pe", lambda e, tt=tt, dc=dc, k=k, b=b: e.matmul(PS[:, b, :], lhsT=OT[:, k, tt * 128:(tt + 1) * 128], rhs=wox[dc][0][:, k, :], start=(k == 0), stop=(k == 7)),
                         r=[("OT", k), wox[dc][1]], w=[psk(b)])
                S.op("dve", lambda e, t=t, dc=dc, b=b: e.tensor_tensor(out=X[:, t, dc * 512:(dc + 1) * 512], in0=PS[:, b, :], in1=X[:, t, dc * 512:(dc + 1) * 512], op=ALU.add),
                     r=[psk(b), ("X", t)], w=[("X", t)])

    xa_q(0)
    for tc in range(4):
        xa_heads(tc)
        if tc + 1 < 4:
            xa_q(tc + 1)
        xa_o(tc)
    A.free("QX0", "QX1", "PX0", "PX1", "PX2", "PX3", "OT", "rsx0", "rsx1", "KX", "VX", "HTx")

    if dbg == "xattn":
        S.op("sp", lambda e: e.dma_start(out=out_v, in_=X), r=[("X", t) for t in range(16)], w=["o1"], dma=True)
        S.op("sp", lambda e: e.nop(), r=["o1"])
        S.emit(nc)
        es.close()
        return nc

    HT = A.alloc("HTm", BF16, [128, 8, 2048], top=True)
    WR = A.alloc("WR", F32, [128, 8, 20])
    wrs = A.alloc("wrs", F32, [128, 8, 20])
    S.op("sp", lambda e: e.dma_start(out=wrs, in_=wr_d.rearrange("(k p) f -> p k f", p=128)), w=["wrs"], dma=True)
    S.op("pool", lambda e: e.tensor_tensor(out=WR, in0=wrs, in1=gT[:, 24:32].unsqueeze(2).broadcast_to([128, 8, 20]), op=ALU.mult), r=["wrs", "gT"], w=["WR"])
    LOG = A.alloc("LOG", F32, [128, 16, 20])
    rstd4 = rms_stats(xtile, 16, 1024, "rstd4")
    Hfs = [A.alloc("Hf%d" % i, F32, [128, 1024]) for i in range(2)]
    HTFs = [A.alloc("HTF%d" % i, F32, [128, 8, 128]) for i in range(2)]

    def moe_A(t):
        p = t % 2
        Hf, hk = Hfs[p], "Hf%d" % p
        S.op("dve", lambda e: e.tensor_scalar(out=Hf, in0=X[:, t, :], scalar1=rstd4[:, t:t + 1], scalar2=None, op0=ALU.mult), r=[("X", t), ("rstd4", t)], w=[hk])
        for k in range(8):
            b = 2 * p + k // 4
            S.op("pe", lambda e, k=k, b=b: e.transpose(out=PS[:, b, (k % 4) * 128:(k % 4 + 1) * 128], in_=Hf[:, k * 128:(k + 1) * 128], identity=idf),
                 r=[hk, "idf"], w=[psk(b)])

    def moe_B(t):
        p = t % 2
        HTF, fk = HTFs[p], "HTF%d" % p
        for bb in range(2):
            b = 2 * p + bb
            S.op("act", lambda e, b=b, bb=bb: e.copy(out=HT[:, 4 * bb:4 * bb + 4, t * 128:(t + 1) * 128], in_=PS[:, b, :].rearrange("p (k c) -> p k c", k=4)),
                 r=[psk(b)], w=[("HTm", t, bb)])
            S.op("dve", lambda e, b=b, bb=bb: e.tensor_copy(out=HTF[:, 4 * bb:4 * bb + 4, :], in_=PS[:, b, :].rearrange("p (k c) -> p k c", k=4)), r=[psk(b)], w=[(fk, bb)])
        rb_ = 4 + p
        for k in range(8):
            S.op("pe", lambda e, k=k: e.matmul(PS[:, rb_, 0:20], lhsT=HTF[:, k, :], rhs=WR[:, k, :], start=(k == 0), stop=(k == 7)), r=[(fk, k // 4), "WR"], w=[psk(rb_)])
        S.op("dve", lambda e: e.tensor_tensor(out=LOG[:, t, :], in0=PS[:, rb_, 0:20], in1=rbt, op=ALU.add), r=[psk(rb_), "rbt"], w=["LOG"])

    moe_A(0)
    for t in range(16):
        if t + 1 < 16:
            moe_A(t + 1)
        moe_B(t)
    if dbg == "moe_r":
        return dbg_finish([(LOG.rearrange("p a b -> p (a b)"), ["LOG"]), (HT[:, 3, 0:512], [("HTm", t, 0) for t in range(4)])])
    A.free("rstd4", "Hf0", "Hf1", "HTF0", "HTF1", "wrs", "WR")
    GATES = A.alloc("GATES", F32, [128, 16, 16])
    RT = A.alloc("RT", F32, [128, 8, 16, 16])
    gl = LOG[:, :, 0:4]
    el = LOG[:, :, 4:20]
    r4 = lambda i: RT[:, i, :, 0:4]
    r1 = lambda i: RT[:, i, :, 0]
    r16 = lambda i: RT[:, i, :, :]

    def rt(eng, fn):
        S.op(eng, fn, r=["LOG", "RT"], w=["RT"])

    rt("dve", lambda e: e.tensor_reduce(out=r1(0), in_=gl, axis=AX.X, op=ALU.max))
    rt("dve", lambda e: e.tensor_tensor(out=r4(1), in0=gl, in1=r1(0).unsqueeze(2).broadcast_to([128, 16, 4]), op=ALU.subtract))
    rt("act", lambda e: e.activation(out=r4(2), in_=r4(1), func=AF.Exp))
    rt("dve", lambda e: e.tensor_reduce(out=r1(3), in_=r4(2), axis=AX.X, op=ALU.add))
    rt("dve", lambda e: e.reciprocal(out=r1(3), in_=r1(3)))
    rt("dve", lambda e: e.tensor_scalar(out=r4(1), in0=r4(1), scalar1=0.0, scalar2=None, op0=ALU.is_ge))
    rt("dve", lambda e: e.tensor_scalar(out=r4(1), in0=r4(1), scalar1=-1.0, scalar2=1e4, op0=ALU.add, op1=ALU.mult))
    rt("dve", lambda e: e.tensor_tensor(out=r16(4).rearrange("p t (g x) -> p t g x", g=4), in0=el.rearrange("p t (g x) -> p t g x", g=4),
                                        in1=r4(1).unsqueeze(3).broadcast_to([128, 16, 4, 4]), op=ALU.add))
    rt("dve", lambda e: e.tensor_reduce(out=r1(0), in_=r16(4), axis=AX.X, op=ALU.max))
    rt("dve", lambda e: e.tensor_tensor(out=r16(5), in0=r16(4), in1=r1(0).unsqueeze(2).broadcast_to([128, 16, 16]), op=ALU.subtract))
    rt("dve", lambda e: e.tensor_scalar(out=r16(6), in0=r16(5), scalar1=0.0, scalar2=-1e4, op0=ALU.is_ge, op1=ALU.mult))
    rt("dve", lambda e: e.tensor_tensor(out=r16(6), in0=r16(6), in1=r16(5), op=ALU.add))
    rt("dve", lambda e: e.tensor_reduce(out=r1(2), in_=r16(6), axis=AX.X, op=ALU.max))
    rt("dve", lambda e: e.tensor_tensor(out=r16(6), in0=r16(5), in1=r1(2).unsqueeze(2).broadcast_to([128, 16, 16]), op=ALU.is_ge))
    rt("act", lambda e: e.activation(out=r16(7), in_=r16(5), func=AF.Exp))
    rt("act", lambda e: e.activation(out=r1(1), in_=r1(2), func=AF.Exp))
    rt("dve", lambda e: e.tensor_scalar(out=r1(1), in0=r1(1), scalar1=1.0, scalar2=None, op0=ALU.add))
    rt("dve", lambda e: e.reciprocal(out=r1(1), in_=r1(1)))
    rt("dve", lambda e: e.tensor_tensor(out=r1(1), in0=r1(1), in1=r1(3), op=ALU.mult))
    rt("dve", lambda e: e.tensor_tensor(out=r16(7), in0=r16(7), in1=r16(6), op=ALU.mult))
    S.op("dve", lambda e: e.tensor_tensor(out=GATES, in0=r16(7), in1=r1(1).unsqueeze(2).broadcast_to([128, 16, 16]), op=ALU.mult), r=["RT"], w=["GATES"])
    if dbg == "moe_g":
        return dbg_finish([(GATES.rearrange("p a b -> p (a b)"), ["GATES"])])
    A.free("RT", "LOG")

    AT = A.alloc("AT", BF16, [128, 4, 2048])
    sg = [A.alloc("sg%d" % i, F32, [128, 512]) for i in range(2)]
    w_close()
    w_open(2, 4)
    htk_all = lambda tc: [("HTm", t, b) for t in range(4 * tc, 4 * tc + 4) for b in range(2)]
    for ex in range(16):
        wgb, wgk = load_w(wg_d[ex].rearrange("(k p) f -> p k f", p=128), 8, 512, gcol=24)
        wub2, wuk2 = load_w(wu_d[ex].rearrange("(k p) f -> p k f", p=128), 8, 512, gcol=24)
        wdb, wdk = load_w(wd_d[ex].rearrange("(k p) f -> p k f", p=128), 4, 1024)
        for tc in range(4):
            for fc in range(4):
                i = (tc * 4 + fc) % 2
                bg, bu = i, 2 + i
                for k in range(8):
                    S.op("pe", lambda e, wgb=wgb, fc=fc, tc=tc, k=k, bg=bg: e.matmul(PS[:, bg, :], lhsT=wgb[:, k, fc * 128:(fc + 1) * 128], rhs=HT[:, k, tc * 512:(tc + 1) * 512],
                                                                                     start=(k == 0), stop=(k == 7)), r=htk_all(tc) + [wgk], w=[psk(bg)])
                for k in range(8):
                    S.op("pe", lambda e, wub2=wub2, fc=fc, tc=tc, k=k, bu=bu: e.matmul(PS[:, bu, :], lhsT=wub2[:, k, fc * 128:(fc + 1) * 128], rhs=HT[:, k, tc * 512:(tc + 1) * 512],
                                                                                       start=(k == 0), stop=(k == 7)), r=htk_all(tc) + [wuk2], w=[psk(bu)])
                S.op("act", lambda e, i=i, bg=bg: e.activation(out=sg[i], in_=PS[:, bg, :], func=AF.Silu), r=[psk(bg)], w=["sg%d" % i])
                S.op("dve", lambda e, i=i, bu=bu, fc=fc, tc=tc: e.tensor_tensor(out=AT[:, fc, tc * 512:(tc + 1) * 512], in0=PS[:, bu, :], in1=sg[i], op=ALU.mult),
                     r=[psk(bu), "sg%d" % i], w=[("AT", fc, tc)])
        for t in range(16):
            for dc in range(2):
                b = 4 + (t * 2 + dc) % 4
                for fk in range(4):
                    S.op("pe", lambda e, wdb=wdb, t=t, dc=dc, fk=fk, b=b: e.matmul(PS[:, b, :], lhsT=AT[:, fk, t * 128:(t + 1) * 128], rhs=wdb[:, fk, dc * 512:(dc + 1) * 512],
                                                                                   start=(fk == 0), stop=(fk == 3)), r=[("AT", fk, t // 4), wdk], w=[psk(b)])
                S.op("dve", lambda e, t=t, dc=dc, b=b, ex=ex: e.scalar_tensor_tensor(out=X[:, t, dc * 512:(dc + 1) * 512], in0=PS[:, b, :], scalar=GATES[:, t, ex:ex + 1],
                                                                                     in1=X[:, t, dc * 512:(dc + 1) * 512], op0=ALU.mult, op1=ALU.add),
                     r=[psk(b), "GATES", ("X", t)], w=[("X", t)])
    w_close()
    A.free("AT", "sg0", "sg1", "HTm", "GATES")

    gfin = A.alloc("gfin", F32, [128, 1024])
    S.op("sp", lambda e: e.dma_start(out=gfin, in_=gfin_d.partition_broadcast(128)), w=["gfin"], dma=True)
    rstd5 = rms_stats(xtile, 16, 1024, "rstd5")
    outk = []
    for t in range(16):
        S.op("dve", lambda e, t=t: e.scalar_tensor_tensor(out=X[:, t, :], in0=X[:, t, :], scalar=rstd5[:, t:t + 1], in1=gfin, op0=ALU.mult, op1=ALU.mult),
             r=[("X", t), ("rstd5", t), "gfin"], w=[("X", t)])
        if t % 4 == 3:
            i = t // 4
            S.op("sp", lambda e, i=i: e.dma_start(out=out_v[:, 4 * i:4 * i + 4, :], in_=X[:, 4 * i:4 * i + 4, :]), r=[("X", tt) for tt in range(4 * i, 4 * i + 4)], w=["out%d" % i], dma=True)
            outk.append("out%d" % i)
    S.op("sp", lambda e: e.nop(), r=outk)
    S.emit(nc)
    es.close()
    build.peak = A.peak
    return nc


def host_consts():
    ident = np.eye(128, dtype=np.float32)
    t = np.arange(2048)
    row = (t // 64).astype(np.float64)
    col = (t % 64).astype(np.float64)
    freqs = 10000.0 ** (-np.arange(16, dtype=np.float64) / 16)
    ar = row[:, None] * freqs[None, :]
    ac = col[:, None] * freqs[None, :]
    cos = np.concatenate([np.cos(ar), np.cos(ar), np.cos(ac), np.cos(ac)], axis=1).astype(np.float32)
    sin = np.concatenate([-np.sin(ar), np.sin(ar), -np.sin(ac), np.sin(ac)], axis=1).astype(np.float32)
    rope = np.stack([cos.reshape(16, 128, 64).transpose(1, 0, 2), sin.reshape(16, 128, 64).transpose(1, 0, 2)]).astype(np.float32)
    mB = np.zeros((128, 2, 4, 2, 16), np.float32)
    for gl in range(2):
        for par in range(2):
            for m in range(4):
                if m % 2 == par:
                    mB[gl * 64:(gl + 1) * 64, par, m, gl, :] = 1.0
    mC = np.zeros((128, 2, 2, 64), np.float32)
    for g8 in range(8):
        for par in range(2):
            for gl in range(2):
                if (g8 % 4) // 2 == par and g8 % 2 == gl:
                    mC[g8 * 16:(g8 + 1) * 16, par, gl, :] = 1.0
    masks = np.stack([mB.reshape(128, 2, 128), mC.reshape(128, 2, 128)]).astype(np.float32)
    mB4 = np.zeros((128, 4, 4, 2, 16), np.float32)
    for gl in range(2):
        for m in range(4):
            mB4[gl * 64:(gl + 1) * 64, m, m, gl, :] = 1.0
    return ident, rope, masks, mB4.reshape(128, 4, 128)


_CACHE = {}


def make_in_maps(inp):
    f = lambda a: np.ascontiguousarray(np.asarray(a, dtype=np.float32))
    ident, rope, masks, masksB = host_consts()
    vecs = np.concatenate([
        f(inp["mix_norm_g"][0]).reshape(8, 128), f(inp["xa_norm_g"][0]).reshape(8, 128), f(inp["mem_norm_g"][0]).reshape(8, 128),
        f(inp["moe_norm_g"][0]).reshape(8, 128), f(inp["att_out_g"][0]).reshape(4, 128), f(inp["ssm_out_g"][0]).reshape(4, 128),
        f(inp["ssm_d"][0]).reshape(4, 128), f(inp["ssm_glu_b"][0]).reshape(4, 128)], axis=0)
    ls = f(inp["ssm_log_step"][0])
    ls_rep = np.repeat(ls.reshape(2, 16, 2, 1), 64, axis=3).reshape(32, 128)
    s5p = np.concatenate([f(inp["ssm_lambda_re"][0]).reshape(32, 128), f(inp["ssm_lambda_im"][0]).reshape(32, 128), ls_rep], axis=0)
    shared = {
        "w_in": f(inp["w_in"][0]), "w_out": f(inp["w_out"][0]), "glu_w": f(inp["ssm_glu_w"][0]),
        "xa_wq": f(inp["xa_wq"][0]), "xa_wk": f(inp["xa_wk"][0]), "xa_wv": f(inp["xa_wv"][0]), "xa_wo": f(inp["xa_wo"][0]),
        "wr": f(np.concatenate([np.asarray(inp["router_grp_w"][0]), np.asarray(inp["router_exp_w"][0])], axis=1)),
        "wg": f(inp["exp_w_gate"][0]), "wu": f(inp["exp_w_up"][0]), "wd": f(inp["exp_w_down"][0]),
        "vecs": f(vecs), "gfin": f(inp["final_norm_g"]), "gq": f(inp["q_norm_g"][0]), "gk": f(inp["k_norm_g"][0]),
        "rb": f(np.concatenate([np.asarray(inp["router_grp_b"][0]), np.asarray(inp["router_exp_b"][0])])),
        "g64": f(np.asarray(inp["att_out_g"][0]).reshape(8, 64).T),
        "s5p": f(s5p), "b_re": f(inp["ssm_b_re"][0]), "b_im": f(inp["ssm_b_im"][0]), "c_re": f(inp["ssm_c_re"][0]), "c_im": f(inp["ssm_c_im"][0]),
        "ident": ident, "rope": rope, "masks": masks, "masksB": masksB,
    }
    x = f(inp["x"])
    mem = f(inp["mem"])
    maps = []
    for b in range(8):
        m = dict(shared)
        m["x"] = x[b]
        m["mem"] = mem[b]
        maps.append(m)
    return maps


def kernel(**inputs):
    if "nc" not in _CACHE:
        _CACHE["nc"] = build()
    nc = _CACHE["nc"]
    in_maps = make_in_maps(inputs)
    res = run_bass_kernel_spmd(nc, in_maps, core_ids=list(range(8)))
    out = np.stack([np.asarray(res.results[b]["out"], dtype=np.float32) for b in range(8)], axis=0)
    return out
```
